# Optimizing a Trainium2 kernel written in Bass

```python
import math
import jax, jax.numpy as jnp
from jax import lax
import numpy as np

D_MODEL = 2048
BATCH = 8
SEQ = 2048
DEPTH = 2

MIX_WIDTH = D_MODEL
CONV_WIDTH = D_MODEL // 4
CONV_GROUPS = 4
CONV_K = 3
SPARSE_HEADS = 6
SPARSE_HEAD_DIM = 128
SPARSE_WIDTH = SPARSE_HEADS * SPARSE_HEAD_DIM
IDX_HEADS = 8
IDX_DIM = 64
INDEX_TOPK_MAX = 256
DIFF_HEADS = 6
DIFF_QK_DIM = 64
DIFF_V_DIM = 2 * DIFF_QK_DIM
DIFF_WIDTH = DIFF_HEADS * DIFF_V_DIM

ROPE_THETA = 10000.0
NORM_EPS = 1e-6
SUBLN_EPS = 1e-5
Q_BLOCK = 128

IN_SPLITS = (CONV_WIDTH, CONV_WIDTH, CONV_WIDTH, CONV_WIDTH,
             SPARSE_WIDTH, SPARSE_HEAD_DIM, SPARSE_HEAD_DIM, IDX_HEADS * IDX_DIM, IDX_DIM, IDX_HEADS, SPARSE_WIDTH,
             2 * DIFF_HEADS * DIFF_QK_DIM, 2 * DIFF_HEADS * DIFF_QK_DIM, DIFF_WIDTH, DIFF_WIDTH)
N_IN = sum(IN_SPLITS)

kernel_name = "hybrid_conv_dsa_diffattn_parallel_heads"


def _rmsnorm(x, g, eps):
    xf = x.astype(jnp.float32)
    y = xf * lax.rsqrt(jnp.mean(xf * xf, axis=-1, keepdims=True) + eps)
    return (y * g.astype(jnp.float32)).astype(x.dtype)


def _rope(x, pos):
    d = x.shape[-1]
    half = d // 2
    inv_freq = jnp.exp(-math.log(ROPE_THETA) * jnp.arange(half, dtype=jnp.float32) * (2.0 / d))
    ang = pos.astype(jnp.float32)[..., None] * inv_freq
    cos = jnp.cos(ang)[:, :, None, :]
    sin = jnp.sin(ang)[:, :, None, :]
    xf = x.astype(jnp.float32)
    x1, x2 = xf[..., :half], xf[..., half:]
    return jnp.concatenate([x1 * cos - x2 * sin, x1 * sin + x2 * cos], axis=-1).astype(x.dtype)


def _split_cols(u):
    offs = np.cumsum(np.array(IN_SPLITS))[:-1].tolist()
    return jnp.split(u, offs, axis=-1)


def _short_conv_mixer(b_gate, c_gate, h, conv_w):
    z = c_gate * h
    s = z.shape[1]
    zp = jnp.pad(z, ((0, 0), (CONV_K - 1, 0), (0, 0)))
    conv = sum(conv_w[j] * zp[:, j:j + s] for j in range(CONV_K))
    return b_gate * conv


def _dsa_mixer(q, k, v, qi, ki, wi, pos):
    bsz, s, _ = q.shape
    q = _rope(q.reshape(bsz, s, SPARSE_HEADS, SPARSE_HEAD_DIM), pos)
    k = _rope(k[:, :, None, :], pos)[:, :, 0]
    qi = _rope(qi.reshape(bsz, s, IDX_HEADS, IDX_DIM), pos)
    ki = _rope(ki[:, :, None, :], pos)[:, :, 0].astype(jnp.float32)
    top_k = min(INDEX_TOPK_MAX, s // 4)
    n_blk = s // Q_BLOCK
    key_pos = jnp.arange(s)
    idx_scale = (IDX_DIM * IDX_HEADS) ** -0.5
    att_scale = SPARSE_HEAD_DIM ** -0.5
    gather = jax.vmap(lambda arr, ids: arr[ids])

    def block(i):
        start = i * Q_BLOCK
        qb = lax.dynamic_slice_in_dim(q, start, Q_BLOCK, axis=1)
        qib = lax.dynamic_slice_in_dim(qi, start, Q_BLOCK, axis=1)
        wib = lax.dynamic_slice_in_dim(wi, start, Q_BLOCK, axis=1)
        qpos = start + jnp.arange(Q_BLOCK)
        logits = jnp.einsum('bqhd,bsd->bqhs', qib.astype(jnp.float32), ki)
        score = jnp.einsum('bqh,bqhs->bqs', wib.astype(jnp.float32), jax.nn.relu(logits)) * idx_scale
        causal = key_pos[None, :] <= qpos[:, None]
        score = jnp.where(causal[None], score, -jnp.inf)
        _, sel = lax.top_k(score, top_k)
        valid = sel <= qpos[None, :, None]
        k_sel = gather(k, sel)
        v_sel = gather(v, sel)
        sc = jnp.einsum('bqhd,bqkd->bqhk', qb, k_sel).astype(jnp.float32) * att_scale
        sc = jnp.where(valid[:, :, None, :], sc, -jnp.inf)
        p = jax.nn.softmax(sc, axis=-1).astype(v.dtype)
        return jnp.einsum('bqhk,bqkd->bqhd', p, v_sel)

    out = lax.map(block, jnp.arange(n_blk))
    return jnp.moveaxis(out, 0, 1).reshape(bsz, s, SPARSE_WIDTH)


def _diff_mixer(q, k, v, pos, lq1, lk1, lq2, lk2, subln_g, lambda_init):
    bsz, s, _ = q.shape
    q = _rope(q.reshape(bsz, s, 2 * DIFF_HEADS, DIFF_QK_DIM), pos).reshape(bsz, s, DIFF_HEADS, 2, DIFF_QK_DIM)
    k = _rope(k.reshape(bsz, s, 2 * DIFF_HEADS, DIFF_QK_DIM), pos).reshape(bsz, s, DIFF_HEADS, 2, DIFF_QK_DIM)
    v = v.reshape(bsz, s, DIFF_HEADS, DIFF_V_DIM)
    lam = (jnp.exp(jnp.sum(lq1.astype(jnp.float32) * lk1.astype(jnp.float32)))
           - jnp.exp(jnp.sum(lq2.astype(jnp.float32) * lk2.astype(jnp.float32))) + lambda_init)
    scale = DIFF_QK_DIM ** -0.5
    key_pos = jnp.arange(s)

    def block(i):
        start = i * Q_BLOCK
        qb = lax.dynamic_slice_in_dim(q, start, Q_BLOCK, axis=1)
        qpos = start + jnp.arange(Q_BLOCK)
        sc = jnp.einsum('bqhcd,bkhcd->bhcqk', qb, k).astype(jnp.float32) * scale
        causal = key_pos[None, :] <= qpos[:, None]
        sc = jnp.where(causal[None, None, None], sc, -jnp.inf)
        a = jax.nn.softmax(sc, axis=-1)
        p = (a[:, :, 0] - lam * a[:, :, 1]).astype(v.dtype)
        return jnp.einsum('bhqk,bkhd->bqhd', p, v)

    o = jnp.moveaxis(lax.map(block, jnp.arange(s // Q_BLOCK)), 0, 1).reshape(bsz, s, DIFF_HEADS, DIFF_V_DIM)
    o = _rmsnorm(o, subln_g, SUBLN_EPS) * (1.0 - lambda_init)
    return o.reshape(bsz, s, DIFF_WIDTH)


def setup_inputs(seed: int = 0) -> dict:
    key = jax.random.key(seed)
    ks = jax.random.split(key, 12)
    x = jax.random.normal(ks[0], (BATCH, SEQ, D_MODEL), jnp.float32)
    offset = jax.random.randint(ks[1], (BATCH, 1), 0, 4096, dtype=jnp.int32)
    positions = offset + jnp.arange(SEQ, dtype=jnp.int32)[None, :]
    norm_w = 1.0 + 0.02 * jax.random.normal(ks[2], (DEPTH, D_MODEL), jnp.float32)
    w_in = jax.random.normal(ks[3], (DEPTH, D_MODEL, N_IN), jnp.float32) * D_MODEL ** -0.5
    conv_w = jax.random.normal(ks[4], (DEPTH, CONV_K, CONV_WIDTH), jnp.float32) * CONV_K ** -0.5
    lam_q1 = 0.1 * jax.random.normal(ks[5], (DEPTH, DIFF_QK_DIM), jnp.float32)
    lam_k1 = 0.1 * jax.random.normal(ks[6], (DEPTH, DIFF_QK_DIM), jnp.float32)
    lam_q2 = 0.1 * jax.random.normal(ks[7], (DEPTH, DIFF_QK_DIM), jnp.float32)
    lam_k2 = 0.1 * jax.random.normal(ks[8], (DEPTH, DIFF_QK_DIM), jnp.float32)
    subln_w = 1.0 + 0.02 * jax.random.normal(ks[9], (DEPTH, DIFF_V_DIM), jnp.float32)
    w_out = jax.random.normal(ks[10], (DEPTH, MIX_WIDTH, D_MODEL), jnp.float32) * MIX_WIDTH ** -0.5
    final_norm_w = 1.0 + 0.02 * jax.random.normal(ks[11], (D_MODEL,), jnp.float32)
    return {"x": x, "positions": positions, "norm_w": norm_w, "w_in": w_in, "conv_w": conv_w,
            "lam_q1": lam_q1, "lam_k1": lam_k1, "lam_q2": lam_q2, "lam_k2": lam_k2,
            "subln_w": subln_w, "w_out": w_out, "final_norm_w": final_norm_w}


def reference(x, positions, norm_w, w_in, conv_w, lam_q1, lam_k1, lam_q2, lam_k2, subln_w, w_out, final_norm_w):
    for layer in range(DEPTH):
        lambda_init = 0.8 - 0.6 * math.exp(-0.3 * layer)
        h = _rmsnorm(x, norm_w[layer], NORM_EPS)
        u = jnp.einsum('bsd,dn->bsn', h, w_in[layer])
        (a_b, a_c, a_h, a_g,
         s_q, s_k, s_v, i_q, i_k, i_w, s_g,
         d_q, d_k, d_v, d_g) = _split_cols(u)
        y_conv = _short_conv_mixer(a_b, a_c, a_h, conv_w[layer]) * jax.nn.silu(a_g)
        y_sparse = _dsa_mixer(s_q, s_k, s_v, i_q, i_k, i_w, positions) * jax.nn.silu(s_g)
        y_diff = _diff_mixer(d_q, d_k, d_v, positions, lam_q1[layer], lam_k1[layer],
                             lam_q2[layer], lam_k2[layer], subln_w[layer], lambda_init) * jax.nn.silu(d_g)
        y = jnp.concatenate([y_conv, y_sparse, y_diff], axis=-1)
        x = x + jnp.einsum('bsm,md->bsd', y, w_out[layer])
    return _rmsnorm(x, final_norm_w, NORM_EPS)
```

```python
import math
import contextlib
import itertools
import numpy as np
import concourse.bass as bass
import concourse.mybir as mybir
from concourse.bass_utils import run_bass_kernel_spmd

F32 = mybir.dt.float32
BF16 = mybir.dt.bfloat16
I32 = mybir.dt.int32
AF = mybir.ActivationFunctionType
ALU = mybir.AluOpType

P = 128
SEQ = 2048
DM = 2048
NT = 16
NTB = 4
DEPTH = 2
N_IN = 7496
NCOLS = 59 * 128 + 8
NORM_EPS = 1e-6
SUBLN_EPS = 1e-5
ROPE_THETA = 10000.0
ESZ = {F32: 4, BF16: 2, I32: 4}
G = 256
NEG_BIG = -1.0e30
MASK_NEG = -30000.0
PI_SAFE = 3.1415925


O_AB, O_AC, O_AH, O_AG = 0, 512, 1024, 1536
O_SQ, O_SK, O_SV, O_IQ, O_IK, O_IW, O_SG = 2048, 2816, 2944, 3072, 3584, 3648, 3656
O_DQ, O_DK, O_DV, O_DG = 4424, 5192, 5960, 6728


def _col_perm():
    cols = []
    names = []

    def add(name, start, n=128):
        cols.append(np.arange(start, start + n))
        names.append(name)

    for j in range(4):
        add(("ac", j), O_AC + 128 * j)
        add(("ah", j), O_AH + 128 * j)
        add(("ab", j), O_AB + 128 * j)
        add(("ag", j), O_AG + 128 * j)
    for h in range(6):
        add(("dq", h), O_DQ + 128 * h)
        add(("dk", h), O_DK + 128 * h)
        add(("dv", h), O_DV + 128 * h)
        add(("dg", h), O_DG + 128 * h)
    for c in range(4):
        add(("iq", c), O_IQ + 128 * c)
    cols.append(np.concatenate([np.arange(O_IK, O_IK + 64), np.arange(O_IK, O_IK + 64)]))
    names.append(("ik", 0))
    for h in range(6):
        add(("sq", h), O_SQ + 128 * h)
    add(("sk", 0), O_SK)
    add(("sv", 0), O_SV)
    for h in range(6):
        add(("sg", h), O_SG + 128 * h)
    add(("iw", 0), O_IW, 8)
    return np.concatenate(cols), names


_COLS, _CHUNKS = _col_perm()
assert _COLS.shape[0] == NCOLS

C_ID, C_SW128, C_SW64, C_TRI, C_NEGC, C_ONES = 0, 128, 256, 384, 512, 640
C_INVF128, C_INVF64, C_SGN128, C_SGN64, C_NHALF = 768, 769, 770, 771, 772
C_PW = 776
NBIS = 14
NCONST = 800


def _consts():
    c = np.zeros((P, NCONST), np.float32)
    i = np.arange(P)
    c[i, C_ID + i] = 1.0
    c[(i + 64) % 128, C_SW128 + i] = 1.0
    c[64 * (i // 64) + ((i % 64) + 32) % 64, C_SW64 + i] = 1.0
    c[:, C_TRI:C_TRI + 128] = (i[:, None] <= i[None, :]).astype(np.float32)
    c[:, C_NEGC:C_NEGC + 128] = np.where(i[None, :] > i[:, None], NEG_BIG, 0.0)
    c[:, C_ONES:C_ONES + 128] = 1.0
    f128 = np.exp(np.float32(-math.log(ROPE_THETA)) * np.arange(64, dtype=np.float32) * np.float32(2.0 / 128)).astype(np.float32)
    f64 = np.exp(np.float32(-math.log(ROPE_THETA)) * np.arange(32, dtype=np.float32) * np.float32(2.0 / 64)).astype(np.float32)
    c[:, C_INVF128] = f128[i % 64]
    c[:, C_INVF64] = f64[i % 32]
    c[:, C_SGN128] = np.where(i < 64, -1.0, 1.0)
    c[:, C_SGN64] = np.where((i % 64) < 32, -1.0, 1.0)
    c[:, C_NHALF] = -0.5
    c[:, C_PW:C_PW + NBIS] = (0.5 ** np.arange(1, NBIS + 1, dtype=np.float64)).astype(np.float32)[None, :]
    return c


LP_CONV, LP_SUB, LP_LAM = 0, 12, 16
NLP = 16 + 256


class View:
    __slots__ = ("ap", "rng")

    def __init__(self, ap, rng):
        self.ap = ap
        self.rng = rng


class Arr:
    def __init__(self, region, dt, boff, shape):
        self.region = region
        self.dt = dt
        self.boff = boff
        self.shape = list(shape)
        n = int(np.prod(shape))
        esz = ESZ[dt]
        assert boff % esz == 0 and boff + n * esz <= region.nbytes, (region.name, boff, shape)
        bsz = region.bsz
        ap = region.t[:, boff // bsz:(boff + n * esz) // bsz]
        if dt != region.dt:
            ap = ap.bitcast(dt)
        if len(shape) == 2:
            ap = ap.rearrange("p (a b) -> p a b", b=shape[1])
        elif len(shape) == 3:
            ap = ap.rearrange("p (a b c) -> p a b c", b=shape[1], c=shape[2])
        self.full = ap

    def __getitem__(self, idx):
        if not isinstance(idx, tuple):
            idx = (idx,)
        idx = list(idx) + [slice(None)] * (1 + len(self.shape) - len(idx))
        sel = []
        for d, ix in enumerate(idx[1:]):
            n = self.shape[d]
            if isinstance(ix, slice):
                lo = 0 if ix.start is None else ix.start
                hi = n if ix.stop is None else ix.stop
            else:
                lo, hi = ix, ix + 1
            assert 0 <= lo < hi <= n, (self.region.name, self.shape, idx)
            sel.append((lo, hi))
        ap = self.full[tuple(idx)]
        esz = ESZ[self.dt]
        shape = self.shape
        k = len(shape) - 1
        while k > 0 and sel[k] == (0, shape[k]):
            k -= 1
        strides = [int(np.prod(shape[i + 1:])) for i in range(len(shape))]
        run = (sel[k][1] - sel[k][0]) * strides[k]
        rng = []
        for comb in itertools.product(*[range(lo, hi) for (lo, hi) in sel[:k]]):
            off = sum(i * s for i, s in zip(comb, strides[:k])) + sel[k][0] * strides[k]
            rng.append((self.region.name, self.boff + off * esz, self.boff + (off + run) * esz))
        return View(ap, rng)


class Region:
    def __init__(self, pg, name, nbytes, kind="sbuf"):
        self.name = name
        self.nbytes = nbytes
        if kind == "sbuf":
            self.dt, self.bsz = BF16, 2
            self.t = pg.es.enter_context(pg.nc.sbuf_tensor(name, [P, nbytes // 2], BF16))
        else:
            self.dt, self.bsz = F32, 4
            self.t = pg.es.enter_context(pg.nc.psum_tensor(name, [P, nbytes // 4], F32))

    def arr(self, dt, boff, shape):
        return Arr(self, dt, boff, shape)


class Prog:
    def __init__(self, nc, es):
        self.nc = nc
        self.es = es
        self.E = {"pe": nc.tensor, "act": nc.scalar, "dve": nc.vector, "pool": nc.gpsimd, "sp": nc.sync}
        self.sem = {}
        self.cnt = {}
        for e in ("pe", "act", "dve", "pool"):
            self.sem[e] = es.enter_context(nc.semaphore("tl_" + e))
            self.cnt[e] = 0
        self.waited = {e: {} for e in self.E}
        self.blocks = {}
        self.n_wait = 0

    def stream(self, name):
        self.sem[name] = self.es.enter_context(self.nc.semaphore("dq_" + name))
        self.cnt[name] = 0

    def _blk(self, rng):
        for (reg, b0, b1) in rng:
            for b in range(b0 // G, (b1 - 1) // G + 1):
                key = (reg, b)
                rec = self.blocks.get(key)
                if rec is None:
                    rec = self.blocks[key] = [None, {}]
                yield rec

    def _sync(self, e, reads, writes, extra=(), embed=False):
        need = {}

        def add(ev):
            if ev is None:
                return
            s, v = ev
            if e == "pe" and s == "pe":
                return
            if need.get(s, 0) < v:
                need[s] = v

        for v in reads:
            for rec in self._blk(v.rng):
                add(rec[0])
        for v in writes:
            for rec in self._blk(v.rng):
                add(rec[0])
                for s, val in rec[1].items():
                    add((s, val))
        for ev in extra:
            add(ev)
        w = self.waited[e]
        todo = [(s, v) for s, v in need.items() if w.get(s, 0) < v]
        emb = None
        if embed and todo:
            todo.sort(key=lambda sv: (sv[0] == e, sv[0]))
            emb = todo.pop(0)
            w[emb[0]] = emb[1]
        for s, v in todo:
            self.E[e].wait_ge(self.sem[s], v)
            w[s] = v
            self.n_wait += 1
        return emb

    def _record(self, ev, reads, writes):
        s, v = ev
        for vw in reads:
            for rec in self._blk(vw.rng):
                if rec[1].get(s, 0) < v:
                    rec[1][s] = v
        for vw in writes:
            for rec in self._blk(vw.rng):
                rec[0] = ev
                rec[1] = {}

    def op(self, e, build, reads=(), writes=(), embed=True):
        emb = self._sync(e, reads, writes, embed=(embed and e != "pe"))
        inst = build(self.E[e])
        if emb is not None:
            inst.wait_op(self.sem[emb[0]], emb[1], "sem-ge")
        self.cnt[e] += 1
        inst.then_inc(self.sem[e], 1)
        self._record((e, self.cnt[e]), reads, writes)

    def dma(self, q, stream, out, in_):
        prev = (stream, self.cnt[stream]) if self.cnt[stream] else None
        self._sync(q, [in_], [out], extra=[prev] if prev else ())
        inst = self.E[q].dma_start(out=out.ap, in_=in_.ap)
        self.cnt[stream] += 16
        inst.then_inc(self.sem[stream], 16)
        self._record((stream, self.cnt[stream]), [in_], [out])

    def wait_all(self, e, views):
        self._sync(e, views, ())


def build_program(layers, final, debug=False):
    L = len(layers)
    nc = bass.Bass("TRN2", target_bir_lowering=False)
    x_d = nc.dram_tensor("x", [SEQ, DM], F32, kind="ExternalInput").ap()
    pos_d = nc.dram_tensor("pos", [P, SEQ], I32, kind="ExternalInput").ap()
    nwb_d = nc.dram_tensor("nwb", [L, P, DM], F32, kind="ExternalInput").ap()
    win_d = nc.dram_tensor("w_in", [L, DM, NCOLS], F32, kind="ExternalInput").ap()
    wout_d = nc.dram_tensor("w_out", [L, DM, DM], F32, kind="ExternalInput").ap()
    lp_d = nc.dram_tensor("lp", [P, L * NLP], F32, kind="ExternalInput").ap()
    cst_d = nc.dram_tensor("cst", [P, NCONST], F32, kind="ExternalInput").ap()
    fnw_d = nc.dram_tensor("fnwb", [P, DM], F32, kind="ExternalInput").ap()
    out_d = nc.dram_tensor("out", [SEQ, DM], F32, kind="ExternalOutput").ap()
    x1_d = nc.dram_tensor("x1s", [SEQ, DM], F32, kind="Internal").ap() if L > 1 else None

    es = contextlib.ExitStack()
    with es:
        pg = Prog(nc, es)
        for s in ("w0", "w1", "w2", "x0", "x1", "st0", "st1", "misc", "wo0", "wo1", "wo2", "wo3"):
            pg.stream(s)

        HT = Region(pg, "HT", 65536)
        YT = Region(pg, "YT", 65536)
        TAB = Region(pg, "TAB", 8192)
        WBF = Region(pg, "WBF", 3 * 4096)
        SR = Region(pg, "SR", 53248)
        CR = Region(pg, "CR", 7936)
        PSR = Region(pg, "PSR", 16384, kind="psum")

        hT = HT.arr(BF16, 0, [16, SEQ])
        yT = YT.arr(BF16, 0, [16, SEQ])
        wout = HT.arr(BF16, 0, [16, DM])
        cosT = TAB.arr(BF16, 0, [SEQ])
        sinT = TAB.arr(BF16, 4096, [SEQ])
        wbf = [WBF.arr(BF16, 4096 * i, [16, 128]) for i in range(3)]

        cstf = CR.arr(F32, 0, [NCONST])
        cb = CR.arr(BF16, 3328, [5 * 128])
        i3 = CR.arr(BF16, 4608, [3, 128])
        lpf = CR.arr(F32, 5376, [L * NLP])
        sm = CR.arr(F32, 7552, [96])
        ident = cb[:, 0:128]
        sw128 = cb[:, 128:256]
        sw64 = cb[:, 256:384]
        tri = cb[:, 384:512]
        ones_b = cb[:, 512:640]
        ones_f = cstf[:, C_ONES:C_ONES + 128]
        negc = cstf[:, C_NEGC:C_NEGC + 128]
        SM_SS, SM_MSE, SM_RSTD, SM_LAM, SM_THR = 0, 16, 32, 48, 64

        def dview(ap, name=None, rows=None):
            if name is None:
                return View(ap, [])
            return View(ap, [(name, rows[0] * G, rows[1] * G)])

        bank_rr = [0]

        def ps(bank, dt, shape):
            return PSR.arr(dt, bank * 2048, shape)

        pg.dma("sp", "misc", cstf[:, :], dview(cst_d[:, :]))
        pg.dma("sp", "misc", lpf[:, :], dview(lp_d[:, :]))
        for k, c0 in enumerate((C_ID, C_SW128, C_SW64, C_TRI, C_ONES)):
            pg.op("dve", lambda e, k=k, c0=c0: e.tensor_copy(out=cb[:, 128 * k:128 * (k + 1)].ap, in_=cstf[:, c0:c0 + 128].ap),
                  [cstf[:, c0:c0 + 128]], [cb[:, 128 * k:128 * (k + 1)]])
        for k in range(3):
            pg.op("dve", lambda e, k=k: e.tensor_copy(out=i3[:, k, :].ap, in_=cstf[:, C_ID:C_ID + 128].ap),
                  [cstf[:, C_ID:C_ID + 128]], [i3[:, k, :]])

        def cs(col):
            return cstf[:, col:col + 1]

        wq = []
        for li in range(L):
            c0 = 0
            for (nm, _) in _CHUNKS:
                n = 8 if nm == "iw" else 128
                wq.append((li, c0, n))
                c0 += n
        wq_next = [0]

        def w_issue():
            i = wq_next[0]
            if i >= len(wq):
                return
            li, c0, n = wq[i]
            slot = i % 3
            src = win_d[li].rearrange("(kc p) n -> p kc n", p=P)[:, :, c0:c0 + n]
            pg.dma("pool", "w%d" % slot, wbf[slot][:, :, 0:n], dview(src))
            wq_next[0] += 1

        w_used = [0]

        def w_take():
            i = w_used[0]
            while wq_next[0] < min(len(wq), i + 3):
                w_issue()
            w_used[0] += 1
            return wbf[i % 3]

        def build_tables(invf_col, sgn_col, tmpi, tmpf):
            pg.dma("sp", "misc", tmpi[:, :], dview(pos_d[:, :]))
            pg.op("dve", lambda e: e.tensor_scalar(out=tmpf[:, :].ap, in0=tmpi[:, :].ap, scalar1=cs(invf_col).ap, scalar2=None, op0=ALU.mult),
                  [tmpi[:, :], cs(invf_col)], [tmpf[:, :]])
            pg.op("dve", lambda e: e.tensor_scalar(out=tmpi[:, :].ap, in0=tmpf[:, :].ap, scalar1=float(1.0 / (2.0 * math.pi)), scalar2=None, op0=ALU.mult),
                  [tmpf[:, :]], [tmpi[:, :]])
            pg.op("dve", lambda e: e.scalar_tensor_tensor(out=tmpf[:, :].ap, in0=tmpi[:, :].ap, scalar=float(-2.0 * math.pi), in1=tmpf[:, :].ap, op0=ALU.mult, op1=ALU.add),
                  [tmpi[:, :], tmpf[:, :]], [tmpf[:, :]])
            pg.op("dve", lambda e: e.tensor_scalar(out=tmpf[:, :].ap, in0=tmpf[:, :].ap, scalar1=-PI_SAFE, scalar2=PI_SAFE, op0=ALU.max, op1=ALU.min),
                  [tmpf[:, :]], [tmpf[:, :]])
            pg.op("act", lambda e: e.activation(out=sinT[:, :].ap, in_=tmpf[:, :].ap, func=AF.Sin, scale=cs(sgn_col).ap),
                  [tmpf[:, :], cs(sgn_col)], [sinT[:, :]])
            tmpa = Arr(tmpi.region, F32, tmpi.boff, tmpi.shape)
            pg.op("dve", lambda e: e.scalar_tensor_tensor(out=tmpa[:, :].ap, in0=tmpf[:, :].ap, scalar=-1.0, in1=tmpf[:, :].ap, op0=ALU.mult, op1=ALU.max),
                  [tmpf[:, :]], [tmpa[:, :]])
            pg.op("act", lambda e: e.activation(out=cosT[:, :].ap, in_=tmpa[:, :].ap, func=AF.Sin, scale=-1.0, bias=float(math.pi / 2.0)),
                  [tmpa[:, :]], [cosT[:, :]])

        defer_q = []

        def flush_defer():
            while defer_q:
                defer_q.pop(0)()

        def inproj_fm(consumer, banks=(0, 1, 2, 3)):
            w = w_take()
            for tb in range(NTB):
                b = banks[bank_rr[0] % len(banks)]
                bank_rr[0] += 1
                pa = ps(b, F32, [512])
                for kc in range(16):
                    pg.op("pe", lambda e, kc=kc: e.matmul(pa[:, :].ap, lhsT=w[:, kc, :].ap, rhs=hT[:, kc, tb * 512:(tb + 1) * 512].ap,
                                                         start=(kc == 0), stop=(kc == 15)),
                          [w[:, kc, :], hT[:, kc, tb * 512:(tb + 1) * 512]], [pa[:, :]])
                flush_defer()
                consumer(tb, pa)

        def inproj_tm(ncols, consumer, banks=(0, 1, 2, 3)):
            w = w_take()
            for g4 in range(4):
                b = banks[bank_rr[0] % len(banks)]
                bank_rr[0] += 1
                pa = ps(b, F32, [4, ncols])
                for q in range(4):
                    tt = g4 * 4 + q
                    for kc in range(16):
                        pg.op("pe", lambda e, kc=kc, q=q, tt=tt: e.matmul(pa[:, q, :].ap, lhsT=hT[:, kc, tt * 128:(tt + 1) * 128].ap,
                                                                         rhs=w[:, kc, 0:ncols].ap, start=(kc == 0), stop=(kc == 15)),
                              [w[:, kc, 0:ncols], hT[:, kc, tt * 128:(tt + 1) * 128]], [pa[:, q, :]])
                    flush_defer()
                consumer(g4, pa)

        def rope_consumer(dst_fn, swm, ub, t1, t2, swbanks):
            def consumer(tb, pa):
                u = ub[tb % 2]
                a1 = t1[tb % 2]
                a2 = t2[tb % 2]
                pg.op("act", lambda e: e.activation(out=u[:, :].ap, in_=pa[:, :].ap, func=AF.Copy), [pa[:, :]], [u[:, :]])
                csl = cosT[:, tb * 512:(tb + 1) * 512]
                ssl = sinT[:, tb * 512:(tb + 1) * 512]
                pg.op("dve", lambda e: e.tensor_tensor(out=a1[:, :].ap, in0=u[:, :].ap, in1=csl.ap, op=ALU.mult), [u[:, :], csl], [a1[:, :]])

                def tail():
                    b = swbanks[tb % len(swbanks)]
                    pw = ps(b, F32, [512])
                    pg.op("pe", lambda e: e.matmul(pw[:, :].ap, lhsT=swm.ap, rhs=u[:, :].ap, start=True, stop=True), [swm, u[:, :]], [pw[:, :]])
                    pg.op("dve", lambda e: e.tensor_tensor(out=a2[:, :].ap, in0=pw[:, :].ap, in1=ssl.ap, op=ALU.mult), [pw[:, :], ssl], [a2[:, :]])
                    dst = dst_fn(tb)
                    if isinstance(dst, View):
                        pg.op("dve", lambda e: e.tensor_tensor(out=dst.ap, in0=a1[:, :].ap, in1=a2[:, :].ap, op=ALU.add), [a1[:, :], a2[:, :]], [dst])
                    else:
                        for (dv_, p0, p1) in dst:
                            pg.op("dve", lambda e, dv_=dv_, p0=p0, p1=p1: e.tensor_tensor(out=dv_.ap, in0=a1[p0:p1, :].ap, in1=a2[p0:p1, :].ap, op=ALU.add),
                                  [a1[:, :], a2[:, :]], [dv_])
                defer_q.append(tail)
            return consumer

        scope_cm = [None]

        def scope(name):
            if scope_cm[0] is not None:
                scope_cm[0].__exit__(None, None, None)
                scope_cm[0] = None
            if name is not None:
                scope_cm[0] = nc.named_scope(name)
                scope_cm[0].__enter__()

        for li, layer in enumerate(layers):
            lambda_init = 0.8 - 0.6 * math.exp(-0.3 * layer)
            scope("L%d_p0" % li)
            is_last = (li == L - 1)
            x_src = x_d if li == 0 else x1_d
            x_src_name = None if li == 0 else "D_x1"
            lpo = li * NLP

            stg = [TAB.arr(F32, 0, [DM]), SR.arr(F32, 0, [DM])]
            nwb = SR.arr(F32, 8192, [DM])
            xsb = [SR.arr(BF16, 16384, [DM]), SR.arr(BF16, 20480, [DM])]
            pg.dma("sp", "misc", nwb[:, :], dview(nwb_d[li]))
            for tt in range(NT):
                st = stg[tt % 2]
                xb = xsb[tt % 2]
                src = dview(x_src[tt * 128:(tt + 1) * 128, :], x_src_name, (tt, tt + 1))
                pg.dma("sp", "x%d" % (tt % 2), st[:, :], src)
                ss = sm[:, SM_SS + tt:SM_SS + tt + 1]
                mse = sm[:, SM_MSE + tt:SM_MSE + tt + 1]
                rstd = sm[:, SM_RSTD + tt:SM_RSTD + tt + 1]
                pg.op("act", lambda e: e.activation(out=xb[:, :].ap, in_=st[:, :].ap, func=AF.Square, accum_out=ss.ap), [st[:, :]], [xb[:, :], ss])
                pg.op("dve", lambda e: e.tensor_scalar(out=mse.ap, in0=ss.ap, scalar1=1.0 / DM, scalar2=NORM_EPS, op0=ALU.mult, op1=ALU.add), [ss], [mse])
                pg.op("pool", lambda e: e.tensor_tensor(out=rstd.ap, in0=mse.ap, in1=cs(C_NHALF).ap, op=ALU.pow), [mse, cs(C_NHALF)], [rstd])
                pg.op("dve", lambda e: e.scalar_tensor_tensor(out=xb[:, :].ap, in0=st[:, :].ap, scalar=rstd.ap, in1=nwb[:, :].ap, op0=ALU.mult, op1=ALU.mult),
                      [st[:, :], rstd, nwb[:, :]], [xb[:, :]])
                for g4 in range(4):
                    b = (tt % 2) * 4 + g4
                    pt = ps(b, BF16, [4, 128])
                    for q in range(4):
                        c = g4 * 4 + q
                        pg.op("pe", lambda e, q=q, c=c: e.transpose(out=pt[:, q, :].ap, in_=xb[:, c * 128:(c + 1) * 128].ap, identity=ident.ap),
                              [xb[:, c * 128:(c + 1) * 128], ident], [pt[:, q, :]])
                    dst = hT[:, g4 * 4:g4 * 4 + 4, tt * 128:(tt + 1) * 128]
                    pg.op("act", lambda e: e.activation(out=dst.ap, in_=pt[:, :, :].ap, func=AF.Copy), [pt[:, :, :]], [dst])

            scope("L%d_tab" % li)
            lamt = SR.arr(F32, 0, [2, 64])
            lsum_a = CR.arr(F32, 7552 + 4 * SM_LAM, [2])
            lexp_a = CR.arr(F32, 7552 + 4 * SM_LAM + 8, [2])
            lsum = lsum_a[:, :]
            lexp = lexp_a[:, :]
            neglam = sm[:, SM_LAM + 4:SM_LAM + 5]
            gsc = sm[:, SM_LAM + 5:SM_LAM + 6]
            lq = lpf[:, lpo + LP_LAM:lpo + LP_LAM + 256]
            for k in range(2):
                a = lpf[:, lpo + LP_LAM + 128 * k:lpo + LP_LAM + 128 * k + 64]
                b_ = lpf[:, lpo + LP_LAM + 128 * k + 64:lpo + LP_LAM + 128 * k + 128]
                pg.op("dve", lambda e, k=k, a=a, b_=b_: e.tensor_tensor(out=lamt[:, k, :].ap, in0=a.ap, in1=b_.ap, op=ALU.mult), [a, b_], [lamt[:, k, :]])
                pg.op("dve", lambda e, k=k: e.reduce_sum(out=lsum_a[:, k:k + 1].ap, in_=lamt[:, k, :].ap, axis=mybir.AxisListType.X),
                      [lamt[:, k, :]], [lsum_a[:, k:k + 1]])
            pg.op("act", lambda e: e.activation(out=lexp.ap, in_=lsum.ap, func=AF.Exp), [lsum], [lexp])
            pg.op("dve", lambda e: e.scalar_tensor_tensor(out=neglam.ap, in0=lexp_a[:, 1:2].ap, scalar=-lambda_init, in1=lexp_a[:, 0:1].ap, op0=ALU.add, op1=ALU.subtract),
                  [lexp], [neglam])
            sub_col = lpf[:, lpo + LP_SUB:lpo + LP_SUB + 1]
            pg.op("dve", lambda e: e.tensor_scalar(out=gsc.ap, in0=sub_col.ap, scalar1=float(1.0 - lambda_init), scalar2=None, op0=ALU.mult), [sub_col], [gsc])

            build_tables(C_INVF64, C_SGN64, SR.arr(I32, 8192, [SEQ]), SR.arr(F32, 16384, [SEQ]))

            scope("L%d_conv" % li)
            cT = SR.arr(F32, 0, [SEQ])
            zp = SR.arr(F32, 8192, [SEQ + 64])
            cv = SR.arr(F32, 8192 + 8448, [SEQ])
            sgt = [SR.arr(F32, 25088, [512]), SR.arr(F32, 27136, [512])]
            for j in range(4):
                pg.op("dve", lambda e: e.memset(zp[:, 0:2].ap, 0.0), [], [zp[:, 0:2]])

                def cons_c(tb, pa):
                    d_ = cT[:, tb * 512:(tb + 1) * 512]
                    pg.op("act", lambda e: e.activation(out=d_.ap, in_=pa[:, :].ap, func=AF.Copy), [pa[:, :]], [d_])
                inproj_fm(cons_c)

                def cons_h(tb, pa, j=j):
                    z_ = zp[:, 2 + tb * 512:2 + (tb + 1) * 512]
                    c_ = cT[:, tb * 512:(tb + 1) * 512]
                    pg.op("dve", lambda e: e.tensor_tensor(out=z_.ap, in0=pa[:, :].ap, in1=c_.ap, op=ALU.mult), [pa[:, :], c_], [z_])
                    o_ = cv[:, tb * 512:(tb + 1) * 512]
                    w0 = lpf[:, lpo + LP_CONV + 3 * j + 0:lpo + LP_CONV + 3 * j + 1]
                    w1 = lpf[:, lpo + LP_CONV + 3 * j + 1:lpo + LP_CONV + 3 * j + 2]
                    w2 = lpf[:, lpo + LP_CONV + 3 * j + 2:lpo + LP_CONV + 3 * j + 3]
                    z0 = zp[:, tb * 512:(tb + 1) * 512]
                    z1 = zp[:, 1 + tb * 512:1 + (tb + 1) * 512]
                    pg.op("dve", lambda e: e.tensor_scalar(out=o_.ap, in0=z0.ap, scalar1=w0.ap, scalar2=None, op0=ALU.mult), [z0, w0], [o_])
                    pg.op("dve", lambda e: e.scalar_tensor_tensor(out=o_.ap, in0=z1.ap, scalar=w1.ap, in1=o_.ap, op0=ALU.mult, op1=ALU.add), [z1, w1, o_], [o_])
                    pg.op("dve", lambda e: e.scalar_tensor_tensor(out=o_.ap, in0=z_.ap, scalar=w2.ap, in1=o_.ap, op0=ALU.mult, op1=ALU.add), [z_, w2, o_], [o_])
                inproj_fm(cons_h)

                def cons_b(tb, pa):
                    o_ = cv[:, tb * 512:(tb + 1) * 512]
                    pg.op("dve", lambda e: e.tensor_tensor(out=o_.ap, in0=pa[:, :].ap, in1=o_.ap, op=ALU.mult), [pa[:, :], o_], [o_])
                inproj_fm(cons_b)

                def cons_g(tb, pa, j=j):
                    s_ = sgt[tb % 2]
                    o_ = cv[:, tb * 512:(tb + 1) * 512]
                    y_ = yT[:, j, tb * 512:(tb + 1) * 512]
                    pg.op("act", lambda e: e.activation(out=s_[:, :].ap, in_=pa[:, :].ap, func=AF.Silu), [pa[:, :]], [s_[:, :]])
                    pg.op("dve", lambda e: e.tensor_tensor(out=y_.ap, in0=s_[:, :].ap, in1=o_.ap, op=ALU.mult), [s_[:, :], o_], [y_])
                inproj_fm(cons_g)

            scope("L%d_diff" % li)
            dqT = SR.arr(BF16, 0, [SEQ])
            dkT = SR.arr(BF16, 4096, [SEQ])
            vh = SR.arr(BF16, 8192, [16, 128])
            gT = SR.arr(F32, 12288, [SEQ])
            ub = [SR.arr(BF16, 20480, [512]), SR.arr(BF16, 21504, [512])]
            t1 = [SR.arr(F32, 22528, [512]), SR.arr(F32, 24576, [512])]
            t2 = [SR.arr(F32, 26624, [512]), SR.arr(F32, 28672, [512])]
            ptb = [SR.arr(BF16, 30720 + 1024 * i, [512]) for i in range(4)]
            rec = SR.arr(F32, 34816, [512])
            r0 = SR.arr(F32, 36864, [512])
            r1 = SR.arr(F32, 38912, [512])
            od = SR.arr(F32, 40960, [512])
            sq = SR.arr(F32, 43008, [512])
            lnr = SR.arr(F32, 45056, [512])
            rsd = SR.arr(F32, 47104, [512])
            dq1T = SR.arr(BF16, 49152, [SEQ])
            pg.op("pool", lambda e: e.memset(dqT[64:128, :].ap, 0.0), [], [dqT[64:128, :]])
            pg.op("pool", lambda e: e.memset(dq1T[0:64, :].ap, 0.0), [], [dq1T[0:64, :]])
            dqc = [dqT, dq1T]
            rr = [r0, r1]
            pending = [None]

            def flush_pending():
                if pending[0] is not None:
                    pending[0]()
                    pending[0] = None

            for h in range(6):
                inproj_fm(rope_consumer(lambda tb: [(dqT[0:64, tb * 512:(tb + 1) * 512], 0, 64), (dq1T[64:128, tb * 512:(tb + 1) * 512], 64, 128)],
                                        sw64, ub, t1, t2, (2, 3)), banks=(0, 1))
                flush_pending()
                inproj_fm(rope_consumer(lambda tb: dkT[:, tb * 512:(tb + 1) * 512], sw64, ub, t1, t2, (2, 3)), banks=(0, 1))

                def cons_v(g4, pa):
                    d_ = vh[:, g4 * 4:g4 * 4 + 4, :]
                    pg.op("act", lambda e: e.activation(out=d_.ap, in_=pa[:, :, :].ap, func=AF.Copy), [pa[:, :, :]], [d_])
                inproj_tm(128, cons_v, banks=(0, 1))

                def cons_dg(tb, pa):
                    d_ = gT[:, tb * 512:(tb + 1) * 512]
                    pg.op("act", lambda e: e.activation(out=d_.ap, in_=pa[:, :].ap, func=AF.Silu), [pa[:, :]], [d_])
                inproj_fm(cons_dg, banks=(0, 1))
                flush_defer()

                for tb in range(NTB):
                    t0 = tb * 512
                    nsc = 4 * tb + 4
                    steps = [(c, sc) for c in range(2) for sc in range(nsc)]

                    def col0_of(sc):
                        j = sc - 4 * tb
                        return 0 if j < 0 else 128 * j

                    def emit_s(i):
                        c, sc = steps[i]
                        col0 = col0_of(sc)
                        pS = ps(1 + (i % 3), F32, [512])
                        kk = dkT[:, sc * 128:(sc + 1) * 128]
                        qq = dqc[c][:, t0 + col0:t0 + 512]
                        pg.op("pe", lambda e: e.matmul(pS[:, col0:512].ap, lhsT=kk.ap, rhs=qq.ap, start=True, stop=True), [kk, qq], [pS[:, col0:512]])
                        pt_ = ptb[i % 4]
                        pg.op("act", lambda e: e.activation(out=pt_[:, col0:512].ap, in_=pS[:, col0:512].ap, func=AF.Exp, scale=0.125),
                              [pS[:, col0:512]], [pt_[:, col0:512]])
                        if sc - 4 * tb >= 0:
                            dd = pt_[:, col0:col0 + 128]
                            pg.op("dve", lambda e: e.tensor_tensor(out=dd.ap, in0=dd.ap, in1=tri.ap, op=ALU.mult), [dd, tri], [dd])

                    def emit_pv(i):
                        c, sc = steps[i]
                        col0 = col0_of(sc)
                        po = ps(4 + c, F32, [512])
                        pd = ps(6 + c, F32, [512])
                        pt_ = ptb[i % 4]
                        vv = vh[:, sc, :]
                        pg.op("pe", lambda e: e.matmul(po[:, col0:512].ap, lhsT=vv.ap, rhs=pt_[:, col0:512].ap, start=(sc == 0), stop=(sc == nsc - 1)),
                              [vv, pt_[:, col0:512]], [po[:, col0:512]])
                        pg.op("pe", lambda e: e.matmul(pd[:, col0:512].ap, lhsT=ones_b.ap, rhs=pt_[:, col0:512].ap, start=(sc == 0), stop=(sc == nsc - 1)),
                              [ones_b, pt_[:, col0:512]], [pd[:, col0:512]])
                        if sc == nsc - 1:
                            def cepi(c=c, po=po, pd=pd):
                                pg.op("dve", lambda e: e.reciprocal(out=rec[:, :].ap, in_=pd[:, :].ap), [pd[:, :]], [rec[:, :]])
                                r_ = rr[c]
                                pg.op("dve", lambda e: e.tensor_tensor(out=r_[:, :].ap, in0=po[:, :].ap, in1=rec[:, :].ap, op=ALU.mult), [po[:, :], rec[:, :]], [r_[:, :]])
                            if tb == 0 and c == 0:
                                cq.append(cepi)
                            else:
                                cepi()

                    cq = []
                    emit_s(0)
                    emit_s(1)
                    for i in range(len(steps)):
                        if i + 2 < len(steps):
                            emit_s(i + 2)
                        if i == len(steps) - 1:
                            while cq:
                                cq.pop(0)()
                        emit_pv(i)
                        if i == 13:
                            flush_pending()
                    pg.op("dve", lambda e: e.scalar_tensor_tensor(out=od[:, :].ap, in0=r1[:, :].ap, scalar=neglam.ap, in1=r0[:, :].ap, op0=ALU.mult, op1=ALU.add),
                          [r1[:, :], neglam, r0[:, :]], [od[:, :]])

                    def epi(h=h, tb=tb, t0=t0):
                        pg.op("act", lambda e: e.activation(out=sq[:, :].ap, in_=od[:, :].ap, func=AF.Square), [od[:, :]], [sq[:, :]])
                        pm = ps(0, F32, [512])
                        pg.op("pe", lambda e: e.matmul(pm[:, :].ap, lhsT=ones_f.ap, rhs=sq[:, :].ap, start=True, stop=True), [ones_f, sq[:, :]], [pm[:, :]])
                        pg.op("dve", lambda e: e.tensor_scalar(out=lnr[:, :].ap, in0=pm[:, :].ap, scalar1=1.0 / 128.0, scalar2=SUBLN_EPS, op0=ALU.mult, op1=ALU.add),
                              [pm[:, :]], [lnr[:, :]])
                        pg.op("act", lambda e: e.activation(out=lnr[:, :].ap, in_=lnr[:, :].ap, func=AF.Ln), [lnr[:, :]], [lnr[:, :]])
                        pg.op("act", lambda e: e.activation(out=rsd[:, :].ap, in_=lnr[:, :].ap, func=AF.Exp, scale=-0.5), [lnr[:, :]], [rsd[:, :]])
                        pg.op("dve", lambda e: e.scalar_tensor_tensor(out=od[:, :].ap, in0=od[:, :].ap, scalar=gsc.ap, in1=rsd[:, :].ap, op0=ALU.mult, op1=ALU.mult),
                              [od[:, :], gsc, rsd[:, :]], [od[:, :]])
                        g_ = gT[:, t0:t0 + 512]
                        y_ = yT[:, 10 + h, t0:t0 + 512]
                        pg.op("dve", lambda e: e.tensor_tensor(out=y_.ap, in0=od[:, :].ap, in1=g_.ap, op=ALU.mult), [od[:, :], g_], [y_])
                    flush_pending()
                    pending[0] = epi
            flush_pending()

            scope("L%d_spin" % li)
            qT = SR.arr(BF16, 0, [6, SEQ])
            iqT = SR.arr(BF16, 24576, [4, SEQ])
            kT = SR.arr(BF16, 40960, [SEQ])
            ikT = SR.arr(BF16, 45056, [SEQ])
            vs = SR.arr(BF16, 49152, [16, 128])
            ub2 = [SR.arr(BF16, 0, [512]), SR.arr(BF16, 1024, [512])]
            t12 = [SR.arr(F32, 2048, [512]), SR.arr(F32, 4096, [512])]
            t22 = [SR.arr(F32, 6144, [512]), SR.arr(F32, 8192, [512])]
            for c in range(4):
                inproj_fm(rope_consumer(lambda tb, c=c: iqT[:, c, tb * 512:(tb + 1) * 512], sw64, ub2, t12, t22, (4, 5)))
            inproj_fm(rope_consumer(lambda tb: ikT[:, tb * 512:(tb + 1) * 512], sw64, ub2, t12, t22, (4, 5)))
            flush_defer()
            build_tables(C_INVF128, C_SGN128, SR.arr(I32, 0, [SEQ]), SR.arr(F32, 8192, [SEQ]))
            ub3 = [YT.arr(BF16, 4 * 4096, [512]), YT.arr(BF16, 4 * 4096 + 1024, [512])]
            t13 = [YT.arr(F32, 4 * 4096 + 2048, [512]), YT.arr(F32, 4 * 4096 + 4096, [512])]
            t23 = [YT.arr(F32, 4 * 4096 + 6144, [512]), YT.arr(F32, 4 * 4096 + 8192, [512])]
            for hh in range(6):
                inproj_fm(rope_consumer(lambda tb, hh=hh: qT[:, hh, tb * 512:(tb + 1) * 512], sw128, ub3, t13, t23, (4, 5)))
            inproj_fm(rope_consumer(lambda tb: kT[:, tb * 512:(tb + 1) * 512], sw128, ub3, t13, t23, (4, 5)))

            def cons_vs(g4, pa):
                d_ = vs[:, g4 * 4:g4 * 4 + 4, :]
                pg.op("act", lambda e: e.activation(out=d_.ap, in_=pa[:, :, :].ap, func=AF.Copy), [pa[:, :, :]], [d_])
            inproj_tm(128, cons_vs)
            for hh in range(6):
                def cons_sg(tb, pa, hh=hh):
                    d_ = yT[:, 4 + hh, tb * 512:(tb + 1) * 512]
                    pg.op("act", lambda e: e.activation(out=d_.ap, in_=pa[:, :].ap, func=AF.Silu), [pa[:, :]], [d_])
                inproj_fm(cons_sg)
            iwa = TAB.arr(F32, 0, [16, 8])

            def cons_iw(g4, pa):
                d_ = iwa[:, g4 * 4:g4 * 4 + 4, :]
                pg.op("act", lambda e: e.activation(out=d_.ap, in_=pa[:, :, :].ap, func=AF.Copy), [pa[:, :, :]], [d_])
            inproj_tm(8, cons_iw)

            flush_defer()
            wo_src = wout_d[li].rearrange("(mc p) n -> p mc n", p=P)
            for mc in range(11, 16):
                pg.dma("pool", "wo%d" % (mc % 4), wout[:, mc, :], dview(wo_src[:, mc, :]))

            scope("L%d_dsa" % li)
            accs = [HT.arr(F32, 0, [SEQ]), HT.arr(F32, 8192, [SEQ])]
            mb = [HT.arr(BF16, 16384, [SEQ]), HT.arr(BF16, 20480, [SEQ])]
            rl = [HT.arr(F32, 24576, [512]), HT.arr(F32, 26624, [512]), HT.arr(F32, 40960, [512]), HT.arr(F32, 43008, [512])]
            ptd = [HT.arr(BF16, 28672 + 768 * i, [3, 128]) for i in range(4)]
            ikhi = HT.arr(BF16, 32768, [SEQ])
            junkb = HT.arr(BF16, 36864, [SEQ])
            recd = TAB.arr(F32, 512, [3, 128])
            tmpd = TAB.arr(F32, 2048, [3, 128])
            bis = TAB.arr(F32, 3584, [64])
            m8 = bis[:, 0:8]
            hi_ = bis[:, 0:1]
            lo_ = bis[:, 8:9]
            rng_ = bis[:, 9:10]
            thr = bis[:, 10:11]
            ncand = bis[:, 11:12]
            ssum = bis[:, 12:13]
            ge_ = bis[:, 13:14]
            pg.op("pool", lambda e: e.memset(ikhi[0:64, :].ap, 0.0), [], [ikhi[0:64, :]])
            pg.op("pool", lambda e: e.tensor_copy(out=ikhi[64:128, :].ap, in_=ikT[64:128, :].ap), [ikT[64:128, :]], [ikhi[64:128, :]])
            pg.op("pool", lambda e: e.memset(ikT[64:128, :].ap, 0.0), [ikhi[64:128, :]], [ikT[64:128, :]])
            ikh = [ikT, ikhi]
            rl_rr = [0]

            def idx_units(qi):
                units = []
                accA = accs[qi % 2]
                nsk = qi + 1
                nkb = (nsk + 3) // 4
                for kb in range(nkb):
                    cols = min(512, 128 * nsk - 512 * kb)
                    for hh in range(8):
                        def u(kb=kb, cols=cols, hh=hh):
                            c, half = hh // 2, hh % 2
                            pL = ps(hh % 2, F32, [512])
                            qv = iqT[:, c, qi * 128:(qi + 1) * 128]
                            kv = ikh[half][:, kb * 512:kb * 512 + cols]
                            pg.op("pe", lambda e: e.matmul(pL[:, 0:cols].ap, lhsT=qv.ap, rhs=kv.ap, start=True, stop=True), [qv, kv], [pL[:, 0:cols]])
                            r_ = rl[rl_rr[0] % 4]
                            rl_rr[0] += 1
                            pg.op("act", lambda e: e.activation(out=r_[:, 0:cols].ap, in_=pL[:, 0:cols].ap, func=AF.Relu), [pL[:, 0:cols]], [r_[:, 0:cols]])
                            a_ = accA[:, kb * 512:kb * 512 + cols]
                            wv = iwa[:, qi, hh:hh + 1]
                            if hh == 0:
                                pg.op("dve", lambda e: e.tensor_scalar(out=a_.ap, in0=r_[:, 0:cols].ap, scalar1=wv.ap, scalar2=None, op0=ALU.mult), [r_[:, 0:cols], wv], [a_])
                            else:
                                pg.op("dve", lambda e: e.scalar_tensor_tensor(out=a_.ap, in0=r_[:, 0:cols].ap, scalar=wv.ap, in1=a_.ap, op0=ALU.mult, op1=ALU.add),
                                      [r_[:, 0:cols], wv, a_], [a_])
                        units.append(u)

                def ud():
                    dg = accA[:, qi * 128:(qi + 1) * 128]
                    pg.op("dve", lambda e: e.tensor_tensor(out=dg.ap, in0=dg.ap, in1=negc.ap, op=ALU.add), [dg, negc], [dg])
                units.append(ud)
                return units

            def bis_units(qi):
                units = []
                accA = accs[qi % 2]
                nk = 128 * (qi + 1)
                av = accA[:, 0:nk]
                mbq = mb[qi % 2]

                def umask():
                    pg.op("dve", lambda e: e.tensor_scalar(out=mbq[:, 0:nk].ap, in0=av.ap, scalar1=thr.ap, scalar2=MASK_NEG, op0=ALU.is_lt, op1=ALU.mult),
                          [av, thr], [mbq[:, 0:nk], junkb[:, :]])
                if qi < 2:
                    def u0():
                        pg.op("dve", lambda e: e.memset(thr.ap, -1.0e29), [], [thr])
                        umask()
                    return [u0]
                a256 = accA[:, 0:256]
                pws = cstf[:, C_PW:C_PW + NBIS]
                rs = bis[:, 16:16 + NBIS]
                jv = junkb[:, 0:nk]

                nrs = bis[:, 36:36 + NBIS]

                def usetup():
                    pg.op("dve", lambda e: e.max(out=m8.ap, in_=av.ap), [av], [m8])
                    pg.op("dve", lambda e: e.tensor_reduce(out=lo_.ap, in_=a256.ap, axis=mybir.AxisListType.X, op=ALU.min), [a256], [lo_])
                    pg.op("dve", lambda e: e.tensor_tensor(out=rng_.ap, in0=hi_.ap, in1=lo_.ap, op=ALU.subtract), [hi_, lo_], [rng_])
                    pg.op("dve", lambda e: e.tensor_scalar(out=rs.ap, in0=pws.ap, scalar1=rng_.ap, scalar2=None, op0=ALU.mult), [pws, rng_], [rs])
                    pg.op("dve", lambda e: e.tensor_scalar(out=nrs.ap, in0=rs.ap, scalar1=-1.0, scalar2=None, op0=ALU.mult), [rs], [nrs])
                    rs0 = bis[:, 16:17]
                    pg.op("dve", lambda e: e.tensor_scalar(out=ncand.ap, in0=lo_.ap, scalar1=rs0.ap, scalar2=-1.0, op0=ALU.add, op1=ALU.mult), [lo_, rs0], [ncand])
                units.append(usetup)
                for it in range(NBIS):
                    def ui(it=it):
                        nrsi = bis[:, 36 + it:37 + it]
                        rsn = bis[:, 17 + it:18 + it] if it + 1 < NBIS else bis[:, 16 + it:17 + it]
                        pg.op("act", lambda e: e.activation(out=jv.ap, in_=av.ap, func=AF.Sign, bias=ncand.ap, scale=1.0, accum_out=ssum.ap),
                              [av, ncand], [ssum])
                        pg.op("dve", lambda e: e.tensor_scalar(out=ge_.ap, in0=ssum.ap, scalar1=float(511.0 - nk), scalar2=nrsi.ap, op0=ALU.is_ge, op1=ALU.mult),
                              [ssum, nrsi], [ge_])
                        pg.op("dve", lambda e: e.scalar_tensor_tensor(out=ncand.ap, in0=ge_.ap, scalar=rsn.ap, in1=ncand.ap, op0=ALU.add, op1=ALU.add),
                              [ge_, rsn, ncand], [ncand])
                    units.append(ui)

                def ufin():
                    pg.op("dve", lambda e: e.tensor_scalar(out=thr.ap, in0=ncand.ap, scalar1=-1.0, scalar2=None, op0=ALU.mult), [ncand], [thr])
                    umask()
                units.append(ufin)
                return units

            def back_units(qi):
                units = []
                nsk = qi + 1
                mbq = mb[qi % 2]
                steps = [(g, sc) for g in range(2) for sc in range(nsk)]

                def emit_s(i):
                    g, sc = steps[i]
                    pS = ps(2 + (i % 2), F32, [3, 128])
                    kk = kT[:, sc * 128:(sc + 1) * 128]
                    qq = qT[:, 3 * g:3 * g + 3, qi * 128:(qi + 1) * 128]
                    mm = mbq[:, sc * 128:(sc + 1) * 128]
                    pg.op("pe", lambda e: e.matmul(pS[:, :, :].ap, lhsT=kk.ap, rhs=qq.ap, start=True, stop=False), [kk, qq], [pS[:, :, :]])
                    pg.op("pe", lambda e: e.matmul(pS[:, :, :].ap, lhsT=mm.ap, rhs=i3[:, :, :].ap, start=False, stop=True), [mm, i3[:, :, :]], [pS[:, :, :]])
                    pt_ = ptd[i % 4]
                    pg.op("act", lambda e: e.activation(out=pt_[:, :, :].ap, in_=pS[:, :, :].ap, func=AF.Exp, scale=float(128.0 ** -0.5)),
                          [pS[:, :, :]], [pt_[:, :, :]])

                def emit_pv(i):
                    g, sc = steps[i]
                    po = ps(4 + g, F32, [3, 128])
                    pd = ps(6 + g, F32, [3, 128])
                    pt_ = ptd[i % 4]
                    vv = vs[:, sc, :]
                    pg.op("pe", lambda e: e.matmul(po[:, :, :].ap, lhsT=vv.ap, rhs=pt_[:, :, :].ap, start=(sc == 0), stop=(sc == nsk - 1)),
                          [vv, pt_[:, :, :]], [po[:, :, :]])
                    pg.op("pe", lambda e: e.matmul(pd[:, :, :].ap, lhsT=ones_b.ap, rhs=pt_[:, :, :].ap, start=(sc == 0), stop=(sc == nsk - 1)),
                          [ones_b, pt_[:, :, :]], [pd[:, :, :]])
                    if sc == nsk - 1:
                        pg.op("act", lambda e: e.activation(out=recd[:, :, :].ap, in_=pd[:, :, :].ap, func=AF.Ln), [pd[:, :, :]], [recd[:, :, :]])
                        pg.op("act", lambda e: e.activation(out=recd[:, :, :].ap, in_=recd[:, :, :].ap, func=AF.Exp, scale=-1.0), [recd[:, :, :]], [recd[:, :, :]])
                        pg.op("dve", lambda e: e.tensor_tensor(out=tmpd[:, :, :].ap, in0=po[:, :, :].ap, in1=recd[:, :, :].ap, op=ALU.mult),
                              [po[:, :, :], recd[:, :, :]], [tmpd[:, :, :]])
                        y_ = yT[:, 4 + 3 * g:4 + 3 * g + 3, qi * 128:(qi + 1) * 128]
                        pg.op("pool", lambda e: e.tensor_tensor(out=y_.ap, in0=tmpd[:, :, :].ap, in1=y_.ap, op=ALU.mult), [tmpd[:, :, :], y_], [y_])

                for i in range(len(steps)):
                    def u(i=i):
                        if i == 0:
                            emit_s(0)
                        if i + 1 < len(steps):
                            emit_s(i + 1)
                        emit_pv(i)
                    units.append(u)
                return units

            def interleave(lists):
                lists = [l for l in lists if l]
                pos = [0] * len(lists)
                for _ in range(sum(len(l) for l in lists)):
                    k = min((k for k in range(len(lists)) if pos[k] < len(lists[k])), key=lambda k: (pos[k] + 0.5) / len(lists[k]))
                    lists[k][pos[k]]()
                    pos[k] += 1

            for r in range(-2, NT):
                streams = []
                if 0 <= r + 2 < NT:
                    streams.append(idx_units(r + 2))
                if 0 <= r + 1 < NT:
                    streams.append(bis_units(r + 1))
                if r >= 0:
                    streams.append(back_units(r))
                interleave(streams)

            if debug and li == 0:
                dbg_d = nc.dram_tensor("dbg_y", [16, P, SEQ], F32, kind="ExternalOutput").ap()
                pg.stream("dbg")
                for c in range(16):
                    pg.dma("pool", "dbg", View(dbg_d[c], []), yT[:, c, :])
            scope("L%d_out" % li)
            for mc in range(0, 11):
                pg.dma("pool", "wo%d" % (mc % 4), wout[:, mc, :], dview(wo_src[:, mc, :]))
            stgo = [SR.arr(F32, 0, [DM]), SR.arr(F32, 8192, [DM])]
            fnw = SR.arr(F32, 16384, [DM])
            junk = SR.arr(BF16, 24576, [DM])
            if is_last and final:
                pg.dma("sp", "misc", fnw[:, :], dview(fnw_d[:, :]))
            for tt in range(NT):
                st = stgo[tt % 2]
                src = dview(x_src[tt * 128:(tt + 1) * 128, :], x_src_name, (tt, tt + 1))
                pg.dma("sp", "x%d" % (tt % 2), st[:, :], src)
                for nb in range(4):
                    pa = ps((tt % 2) * 4 + nb, F32, [512])
                    for mc in range(16):
                        yv = yT[:, mc, tt * 128:(tt + 1) * 128]
                        wv = wout[:, mc, nb * 512:(nb + 1) * 512]
                        pg.op("pe", lambda e, mc=mc: e.matmul(pa[:, :].ap, lhsT=yv.ap, rhs=wv.ap, start=(mc == 0), stop=(mc == 15)), [yv, wv], [pa[:, :]])
                    s_ = st[:, nb * 512:(nb + 1) * 512]
                    pg.op("dve", lambda e: e.tensor_tensor(out=s_.ap, in0=pa[:, :].ap, in1=s_.ap, op=ALU.add), [pa[:, :], s_], [s_])
                if is_last and final:
                    ss = sm[:, SM_SS + tt:SM_SS + tt + 1]
                    mse = sm[:, SM_MSE + tt:SM_MSE + tt + 1]
                    rstd = sm[:, SM_RSTD + tt:SM_RSTD + tt + 1]
                    pg.op("act", lambda e: e.activation(out=junk[:, :].ap, in_=st[:, :].ap, func=AF.Square, accum_out=ss.ap), [st[:, :]], [ss])
                    pg.op("dve", lambda e: e.tensor_scalar(out=mse.ap, in0=ss.ap, scalar1=1.0 / DM, scalar2=NORM_EPS, op0=ALU.mult, op1=ALU.add), [ss], [mse])
                    pg.op("pool", lambda e: e.tensor_tensor(out=rstd.ap, in0=mse.ap, in1=cs(C_NHALF).ap, op=ALU.pow), [mse, cs(C_NHALF)], [rstd])
                    pg.op("dve", lambda e: e.scalar_tensor_tensor(out=st[:, :].ap, in0=st[:, :].ap, scalar=rstd.ap, in1=fnw[:, :].ap, op0=ALU.mult, op1=ALU.mult),
                          [st[:, :], rstd, fnw[:, :]], [st[:, :]])
                if is_last:
                    dst = dview(out_d[tt * 128:(tt + 1) * 128, :], "D_out", (tt, tt + 1))
                else:
                    dst = dview(x1_d[tt * 128:(tt + 1) * 128, :], "D_x1", (tt, tt + 1))
                pg.dma("sp", "st%d" % (tt % 2), dst, st[:, :])

        scope(None)
        for s in ("st0", "st1"):
            nc.sync.wait_ge(pg.sem[s], pg.cnt[s])
        print("[kernel] instr counts", {k: v for k, v in pg.cnt.items()}, "waits", pg.n_wait, flush=True)
    return nc


_PROG_CACHE = {}


def _get_prog(key):
    if key not in _PROG_CACHE:
        layers, final = key
        _PROG_CACHE[key] = build_program(list(layers), final)
    return _PROG_CACHE[key]


def _layer_inputs(layers, norm_w, w_in, conv_w, lam_q1, lam_k1, lam_q2, lam_k2, subln_w, w_out):
    L = len(layers)
    nwb = np.ascontiguousarray(np.broadcast_to(norm_w[layers][:, None, :], (L, P, DM))).astype(np.float32)
    win = np.ascontiguousarray(w_in[layers][:, :, _COLS])
    wout = np.ascontiguousarray(w_out[layers])
    lp = np.zeros((P, L * NLP), np.float32)
    for i, l in enumerate(layers):
        o = i * NLP
        lp[:, o + LP_CONV:o + LP_CONV + 12] = conv_w[l].reshape(3, 4, P).transpose(2, 1, 0).reshape(P, 12)
        lp[:, o + LP_SUB] = subln_w[l]
        lp[:, o + LP_LAM + 0:o + LP_LAM + 64] = lam_q1[l][None, :]
        lp[:, o + LP_LAM + 64:o + LP_LAM + 128] = lam_k1[l][None, :]
        lp[:, o + LP_LAM + 128:o + LP_LAM + 192] = lam_q2[l][None, :]
        lp[:, o + LP_LAM + 192:o + LP_LAM + 256] = lam_k2[l][None, :]
    return nwb, win, wout, lp


def _run(layers, final, xs, positions, norm_w, w_in, conv_w, lam_q1, lam_k1, lam_q2, lam_k2, subln_w, w_out, final_norm_w):
    nc = _get_prog((tuple(layers), final))
    nwb, win, wout, lp = _layer_inputs(layers, norm_w, w_in, conv_w, lam_q1, lam_k1, lam_q2, lam_k2, subln_w, w_out)
    cst = _consts()
    fnwb = np.ascontiguousarray(np.broadcast_to(final_norm_w[None, :], (P, DM))).astype(np.float32)
    in_maps = []
    for b in range(8):
        in_maps.append({
            "x": np.ascontiguousarray(xs[b]),
            "pos": np.ascontiguousarray(np.broadcast_to(positions[b][None, :], (P, SEQ))).astype(np.int32),
            "nwb": nwb, "w_in": win, "w_out": wout, "lp": lp, "cst": cst, "fnwb": fnwb,
        })
    res = run_bass_kernel_spmd(nc, in_maps, core_ids=list(range(8)))
    return np.stack([np.asarray(r["out"]) for r in res.results], axis=0)


FUSED = True


def kernel(x, positions, norm_w, w_in, conv_w, lam_q1, lam_k1, lam_q2, lam_k2, subln_w, w_out, final_norm_w):
    args = [np.asarray(a) for a in (positions, norm_w, w_in, conv_w, lam_q1, lam_k1, lam_q2, lam_k2, subln_w, w_out, final_norm_w)]
    x = np.asarray(x, dtype=np.float32)
    if FUSED:
        out = _run([0, 1], True, x, *args)
    else:
        x1 = _run([0], False, x, *args)
        out = _run([1], True, x1, *args)
    return out.astype(np.float32)
```

```python
import math
import contextlib
import itertools
import numpy as np
import concourse.bass as bass
import concourse.mybir as mybir
from concourse.bass_utils import run_bass_kernel_spmd

F32 = mybir.dt.float32
BF16 = mybir.dt.bfloat16
I32 = mybir.dt.int32
AF = mybir.ActivationFunctionType
ALU = mybir.AluOpType

P = 128
SEQ = 2048
DM = 2048
NT = 16
NTB = 4
DEPTH = 2
N_IN = 7496
NCOLS = 59 * 128 + 8
NORM_EPS = 1e-6
SUBLN_EPS = 1e-5
ROPE_THETA = 10000.0
ESZ = {F32: 4, BF16: 2, I32: 4}
G = 256
NEG_BIG = -1.0e30
MASK_NEG = -30000.0
PI_SAFE = 3.1415925


O_AB, O_AC, O_AH, O_AG = 0, 512, 1024, 1536
O_SQ, O_SK, O_SV, O_IQ, O_IK, O_IW, O_SG = 2048, 2816, 2944, 3072, 3584, 3648, 3656
O_DQ, O_DK, O_DV, O_DG = 4424, 5192, 5960, 6728


def _col_perm():
    cols = []
    names = []

    def add(name, start, n=128):
        cols.append(np.arange(start, start + n))
        names.append(name)

    for j in range(4):
        add(("ac", j), O_AC + 128 * j)
        add(("ah", j), O_AH + 128 * j)
        add(("ab", j), O_AB + 128 * j)
        add(("ag", j), O_AG + 128 * j)
    for h in range(6):
        add(("dq", h), O_DQ + 128 * h)
        add(("dk", h), O_DK + 128 * h)
        add(("dv", h), O_DV + 128 * h)
        add(("dg", h), O_DG + 128 * h)
    for c in range(4):
        add(("iq", c), O_IQ + 128 * c)
    cols.append(np.concatenate([np.arange(O_IK, O_IK + 64), np.arange(O_IK, O_IK + 64)]))
    names.append(("ik", 0))
    for h in range(6):
        add(("sq", h), O_SQ + 128 * h)
    add(("sk", 0), O_SK)
    add(("sv", 0), O_SV)
    for h in range(6):
        add(("sg", h), O_SG + 128 * h)
    add(("iw", 0), O_IW, 8)
    return np.concatenate(cols), names


_COLS, _CHUNKS = _col_perm()
assert _COLS.shape[0] == NCOLS

C_ID, C_SW128, C_SW64, C_TRI, C_NEGC, C_ONES = 0, 128, 256, 384, 512, 640
C_INVF128, C_INVF64, C_SGN128, C_SGN64, C_NHALF = 768, 769, 770, 771, 772
C_PW = 776
NBIS = 14
NCONST = 800


def _consts():
    c = np.zeros((P, NCONST), np.float32)
    i = np.arange(P)
    c[i, C_ID + i] = 1.0
    c[(i + 64) % 128, C_SW128 + i] = 1.0
    c[64 * (i // 64) + ((i % 64) + 32) % 64, C_SW64 + i] = 1.0
    c[:, C_TRI:C_TRI + 128] = (i[:, None] <= i[None, :]).astype(np.float32)
    c[:, C_NEGC:C_NEGC + 128] = np.where(i[None, :] > i[:, None], NEG_BIG, 0.0)
    c[:, C_ONES:C_ONES + 128] = 1.0
    f128 = np.exp(np.float32(-math.log(ROPE_THETA)) * np.arange(64, dtype=np.float32) * np.float32(2.0 / 128)).astype(np.float32)
    f64 = np.exp(np.float32(-math.log(ROPE_THETA)) * np.arange(32, dtype=np.float32) * np.float32(2.0 / 64)).astype(np.float32)
    c[:, C_INVF128] = f128[i % 64]
    c[:, C_INVF64] = f64[i % 32]
    c[:, C_SGN128] = np.where(i < 64, -1.0, 1.0)
    c[:, C_SGN64] = np.where((i % 64) < 32, -1.0, 1.0)
    c[:, C_NHALF] = -0.5
    c[:, C_PW:C_PW + NBIS] = (0.5 ** np.arange(1, NBIS + 1, dtype=np.float64)).astype(np.float32)[None, :]
    return c


LP_CONV, LP_SUB, LP_LAM = 0, 12, 16
NLP = 16 + 256


class View:
    __slots__ = ("ap", "rng")

    def __init__(self, ap, rng):
        self.ap = ap
        self.rng = rng


class Arr:
    def __init__(self, region, dt, boff, shape):
        self.region = region
        self.dt = dt
        self.boff = boff
        self.shape = list(shape)
        n = int(np.prod(shape))
        esz = ESZ[dt]
        assert boff % esz == 0 and boff + n * esz <= region.nbytes, (region.name, boff, shape)
        bsz = region.bsz
        ap = region.t[:, boff // bsz:(boff + n * esz) // bsz]
        if dt != region.dt:
            ap = ap.bitcast(dt)
        if len(shape) == 2:
            ap = ap.rearrange("p (a b) -> p a b", b=shape[1])
        elif len(shape) == 3:
            ap = ap.rearrange("p (a b c) -> p a b c", b=shape[1], c=shape[2])
        self.full = ap

    def __getitem__(self, idx):
        if not isinstance(idx, tuple):
            idx = (idx,)
        idx = list(idx) + [slice(None)] * (1 + len(self.shape) - len(idx))
        sel = []
        for d, ix in enumerate(idx[1:]):
            n = self.shape[d]
            if isinstance(ix, slice):
                lo = 0 if ix.start is None else ix.start
                hi = n if ix.stop is None else ix.stop
            else:
                lo, hi = ix, ix + 1
            assert 0 <= lo < hi <= n, (self.region.name, self.shape, idx)
            sel.append((lo, hi))
        ap = self.full[tuple(idx)]
        esz = ESZ[self.dt]
        shape = self.shape
        k = len(shape) - 1
        while k > 0 and sel[k] == (0, shape[k]):
            k -= 1
        strides = [int(np.prod(shape[i + 1:])) for i in range(len(shape))]
        run = (sel[k][1] - sel[k][0]) * strides[k]
        rng = []
        for comb in itertools.product(*[range(lo, hi) for (lo, hi) in sel[:k]]):
            off = sum(i * s for i, s in zip(comb, strides[:k])) + sel[k][0] * strides[k]
            rng.append((self.region.name, self.boff + off * esz, self.boff + (off + run) * esz))
        return View(ap, rng)


class Region:
    def __init__(self, pg, name, nbytes, kind="sbuf"):
        self.name = name
        self.nbytes = nbytes
        if kind == "sbuf":
            self.dt, self.bsz = BF16, 2
            self.t = pg.es.enter_context(pg.nc.sbuf_tensor(name, [P, nbytes // 2], BF16))
        else:
            self.dt, self.bsz = F32, 4
            self.t = pg.es.enter_context(pg.nc.psum_tensor(name, [P, nbytes // 4], F32))

    def arr(self, dt, boff, shape):
        return Arr(self, dt, boff, shape)


class Prog:
    def __init__(self, nc, es):
        self.nc = nc
        self.es = es
        self.E = {"pe": nc.tensor, "act": nc.scalar, "dve": nc.vector, "pool": nc.gpsimd, "sp": nc.sync}
        self.sem = {}
        self.cnt = {}
        for e in ("pe", "act", "dve", "pool"):
            self.sem[e] = es.enter_context(nc.semaphore("tl_" + e))
            self.cnt[e] = 0
        self.waited = {e: {} for e in self.E}
        self.blocks = {}
        self.n_wait = 0

    def stream(self, name):
        self.sem[name] = self.es.enter_context(self.nc.semaphore("dq_" + name))
        self.cnt[name] = 0

    def _blk(self, rng):
        for (reg, b0, b1) in rng:
            for b in range(b0 // G, (b1 - 1) // G + 1):
                key = (reg, b)
                rec = self.blocks.get(key)
                if rec is None:
                    rec = self.blocks[key] = [None, {}]
                yield rec

    def _sync(self, e, reads, writes, extra=(), embed=False):
        need = {}

        def add(ev):
            if ev is None:
                return
            s, v = ev
            if e == "pe" and s == "pe":
                return
            if need.get(s, 0) < v:
                need[s] = v

        for v in reads:
            for rec in self._blk(v.rng):
                add(rec[0])
        for v in writes:
            for rec in self._blk(v.rng):
                add(rec[0])
                for s, val in rec[1].items():
                    add((s, val))
        for ev in extra:
            add(ev)
        w = self.waited[e]
        todo = [(s, v) for s, v in need.items() if w.get(s, 0) < v]
        emb = None
        if embed and todo:
            todo.sort(key=lambda sv: (sv[0] == e, sv[0]))
            emb = todo.pop(0)
            w[emb[0]] = emb[1]
        for s, v in todo:
            self.E[e].wait_ge(self.sem[s], v)
            w[s] = v
            self.n_wait += 1
        return emb

    def _record(self, ev, reads, writes):
        s, v = ev
        for vw in reads:
            for rec in self._blk(vw.rng):
                if rec[1].get(s, 0) < v:
                    rec[1][s] = v
        for vw in writes:
            for rec in self._blk(vw.rng):
                rec[0] = ev
                rec[1] = {}

    def op(self, e, build, reads=(), writes=(), embed=True):
        emb = self._sync(e, reads, writes, embed=(embed and e != "pe"))
        inst = build(self.E[e])
        if emb is not None:
            inst.wait_op(self.sem[emb[0]], emb[1], "sem-ge")
        self.cnt[e] += 1
        inst.then_inc(self.sem[e], 1)
        self._record((e, self.cnt[e]), reads, writes)

    def dma(self, q, stream, out, in_):
        prev = (stream, self.cnt[stream]) if self.cnt[stream] else None
        self._sync(q, [in_], [out], extra=[prev] if prev else ())
        inst = self.E[q].dma_start(out=out.ap, in_=in_.ap)
        self.cnt[stream] += 16
        inst.then_inc(self.sem[stream], 16)
        self._record((stream, self.cnt[stream]), [in_], [out])

    def wait_all(self, e, views):
        self._sync(e, views, ())


def build_program(layers, final, debug=False):
    L = len(layers)
    nc = bass.Bass("TRN2", target_bir_lowering=False)
    x_d = nc.dram_tensor("x", [SEQ, DM], F32, kind="ExternalInput").ap()
    pos_d = nc.dram_tensor("pos", [P, SEQ], I32, kind="ExternalInput").ap()
    nwb_d = nc.dram_tensor("nwb", [L, P, DM], F32, kind="ExternalInput").ap()
    win_d = nc.dram_tensor("w_in", [L, DM, NCOLS], F32, kind="ExternalInput").ap()
    wout_d = nc.dram_tensor("w_out", [L, DM, DM], F32, kind="ExternalInput").ap()
    lp_d = nc.dram_tensor("lp", [P, L * NLP], F32, kind="ExternalInput").ap()
    cst_d = nc.dram_tensor("cst", [P, NCONST], F32, kind="ExternalInput").ap()
    fnw_d = nc.dram_tensor("fnwb", [P, DM], F32, kind="ExternalInput").ap()
    out_d = nc.dram_tensor("out", [SEQ, DM], F32, kind="ExternalOutput").ap()
    x1_d = nc.dram_tensor("x1s", [SEQ, DM], F32, kind="Internal").ap() if L > 1 else None

    es = contextlib.ExitStack()
    with es:
        pg = Prog(nc, es)
        for s in ("w0", "w1", "w2", "x0", "x1", "st0", "st1", "misc", "wo0", "wo1", "wo2", "wo3"):
            pg.stream(s)

        HT = Region(pg, "HT", 65536)
        YT = Region(pg, "YT", 65536)
        TAB = Region(pg, "TAB", 8192)
        WBF = Region(pg, "WBF", 3 * 4096)
        SR = Region(pg, "SR", 53248)
        CR = Region(pg, "CR", 7936)
        PSR = Region(pg, "PSR", 16384, kind="psum")

        hT = HT.arr(BF16, 0, [16, SEQ])
        yT = YT.arr(BF16, 0, [16, SEQ])
        wout = HT.arr(BF16, 0, [16, DM])
        cosT = TAB.arr(BF16, 0, [SEQ])
        sinT = TAB.arr(BF16, 4096, [SEQ])
        wbf = [WBF.arr(BF16, 4096 * i, [16, 128]) for i in range(3)]

        cstf = CR.arr(F32, 0, [NCONST])
        cb = CR.arr(BF16, 3328, [5 * 128])
        i3 = CR.arr(BF16, 4608, [3, 128])
        lpf = CR.arr(F32, 5376, [L * NLP])
        sm = CR.arr(F32, 7552, [96])
        ident = cb[:, 0:128]
        sw128 = cb[:, 128:256]
        sw64 = cb[:, 256:384]
        tri = cb[:, 384:512]
        ones_b = cb[:, 512:640]
        ones_f = cstf[:, C_ONES:C_ONES + 128]
        negc = cstf[:, C_NEGC:C_NEGC + 128]
        SM_SS, SM_MSE, SM_RSTD, SM_LAM, SM_THR = 0, 16, 32, 48, 64

        def dview(ap, name=None, rows=None):
            if name is None:
                return View(ap, [])
            return View(ap, [(name, rows[0] * G, rows[1] * G)])

        bank_rr = [0]

        def ps(bank, dt, shape):
            return PSR.arr(dt, bank * 2048, shape)

        pg.dma("sp", "misc", cstf[:, :], dview(cst_d[:, :]))
        pg.dma("sp", "misc", lpf[:, :], dview(lp_d[:, :]))
        for k, c0 in enumerate((C_ID, C_SW128, C_SW64, C_TRI, C_ONES)):
            pg.op("dve", lambda e, k=k, c0=c0: e.tensor_copy(out=cb[:, 128 * k:128 * (k + 1)].ap, in_=cstf[:, c0:c0 + 128].ap),
                  [cstf[:, c0:c0 + 128]], [cb[:, 128 * k:128 * (k + 1)]])
        for k in range(3):
            pg.op("dve", lambda e, k=k: e.tensor_copy(out=i3[:, k, :].ap, in_=cstf[:, C_ID:C_ID + 128].ap),
                  [cstf[:, C_ID:C_ID + 128]], [i3[:, k, :]])

        def cs(col):
            return cstf[:, col:col + 1]

        wq = []
        for li in range(L):
            c0 = 0
            for (nm, _) in _CHUNKS:
                n = 8 if nm == "iw" else 128
                wq.append((li, c0, n))
                c0 += n
        wq_next = [0]

        def w_issue():
            i = wq_next[0]
            if i >= len(wq):
                return
            li, c0, n = wq[i]
            slot = i % 3
            src = win_d[li].rearrange("(kc p) n -> p kc n", p=P)[:, :, c0:c0 + n]
            pg.dma("pool", "w%d" % slot, wbf[slot][:, :, 0:n], dview(src))
            wq_next[0] += 1

        w_used = [0]

        def w_take():
            i = w_used[0]
            while wq_next[0] < min(len(wq), i + 3):
                w_issue()
            w_used[0] += 1
            return wbf[i % 3]

        def build_tables(invf_col, sgn_col, tmpi, tmpf):
            pg.dma("sp", "misc", tmpi[:, :], dview(pos_d[:, :]))
            pg.op("dve", lambda e: e.tensor_scalar(out=tmpf[:, :].ap, in0=tmpi[:, :].ap, scalar1=cs(invf_col).ap, scalar2=None, op0=ALU.mult),
                  [tmpi[:, :], cs(invf_col)], [tmpf[:, :]])
            pg.op("dve", lambda e: e.tensor_scalar(out=tmpi[:, :].ap, in0=tmpf[:, :].ap, scalar1=float(1.0 / (2.0 * math.pi)), scalar2=None, op0=ALU.mult),
                  [tmpf[:, :]], [tmpi[:, :]])
            pg.op("dve", lambda e: e.scalar_tensor_tensor(out=tmpf[:, :].ap, in0=tmpi[:, :].ap, scalar=float(-2.0 * math.pi), in1=tmpf[:, :].ap, op0=ALU.mult, op1=ALU.add),
                  [tmpi[:, :], tmpf[:, :]], [tmpf[:, :]])
            pg.op("dve", lambda e: e.tensor_scalar(out=tmpf[:, :].ap, in0=tmpf[:, :].ap, scalar1=-PI_SAFE, scalar2=PI_SAFE, op0=ALU.max, op1=ALU.min),
                  [tmpf[:, :]], [tmpf[:, :]])
            pg.op("act", lambda e: e.activation(out=sinT[:, :].ap, in_=tmpf[:, :].ap, func=AF.Sin, scale=cs(sgn_col).ap),
                  [tmpf[:, :], cs(sgn_col)], [sinT[:, :]])
            tmpa = Arr(tmpi.region, F32, tmpi.boff, tmpi.shape)
            pg.op("dve", lambda e: e.scalar_tensor_tensor(out=tmpa[:, :].ap, in0=tmpf[:, :].ap, scalar=-1.0, in1=tmpf[:, :].ap, op0=ALU.mult, op1=ALU.max),
                  [tmpf[:, :]], [tmpa[:, :]])
            pg.op("act", lambda e: e.activation(out=cosT[:, :].ap, in_=tmpa[:, :].ap, func=AF.Sin, scale=-1.0, bias=float(math.pi / 2.0)),
                  [tmpa[:, :]], [cosT[:, :]])

        defer_q = []

        def flush_defer():
            while defer_q:
                defer_q.pop(0)()

        def inproj_fm(consumer, banks=(0, 1, 2, 3)):
            w = w_take()
            for tb in range(NTB):
                b = banks[bank_rr[0] % len(banks)]
                bank_rr[0] += 1
                pa = ps(b, F32, [512])
                for kc in range(16):
                    pg.op("pe", lambda e, kc=kc: e.matmul(pa[:, :].ap, lhsT=w[:, kc, :].ap, rhs=hT[:, kc, tb * 512:(tb + 1) * 512].ap,
                                                         start=(kc == 0), stop=(kc == 15)),
                          [w[:, kc, :], hT[:, kc, tb * 512:(tb + 1) * 512]], [pa[:, :]])
                flush_defer()
                consumer(tb, pa)

        def inproj_tm(ncols, consumer, banks=(0, 1, 2, 3)):
            w = w_take()
            for g4 in range(4):
                b = banks[bank_rr[0] % len(banks)]
                bank_rr[0] += 1
                pa = ps(b, F32, [4, ncols])
                for q in range(4):
                    tt = g4 * 4 + q
                    for kc in range(16):
                        pg.op("pe", lambda e, kc=kc, q=q, tt=tt: e.matmul(pa[:, q, :].ap, lhsT=hT[:, kc, tt * 128:(tt + 1) * 128].ap,
                                                                         rhs=w[:, kc, 0:ncols].ap, start=(kc == 0), stop=(kc == 15)),
                              [w[:, kc, 0:ncols], hT[:, kc, tt * 128:(tt + 1) * 128]], [pa[:, q, :]])
                    flush_defer()
                consumer(g4, pa)

        def rope_consumer(dst_fn, swm, ub, t1, t2, swbanks):
            def consumer(tb, pa):
                u = ub[tb % 2]
                a1 = t1[tb % 2]
                a2 = t2[tb % 2]
                pg.op("act", lambda e: e.activation(out=u[:, :].ap, in_=pa[:, :].ap, func=AF.Copy), [pa[:, :]], [u[:, :]])
                csl = cosT[:, tb * 512:(tb + 1) * 512]
                ssl = sinT[:, tb * 512:(tb + 1) * 512]
                pg.op("dve", lambda e: e.tensor_tensor(out=a1[:, :].ap, in0=u[:, :].ap, in1=csl.ap, op=ALU.mult), [u[:, :], csl], [a1[:, :]])

                def tail():
                    b = swbanks[tb % len(swbanks)]
                    pw = ps(b, F32, [512])
                    pg.op("pe", lambda e: e.matmul(pw[:, :].ap, lhsT=swm.ap, rhs=u[:, :].ap, start=True, stop=True), [swm, u[:, :]], [pw[:, :]])
                    pg.op("dve", lambda e: e.tensor_tensor(out=a2[:, :].ap, in0=pw[:, :].ap, in1=ssl.ap, op=ALU.mult), [pw[:, :], ssl], [a2[:, :]])
                    dst = dst_fn(tb)
                    if isinstance(dst, View):
                        pg.op("dve", lambda e: e.tensor_tensor(out=dst.ap, in0=a1[:, :].ap, in1=a2[:, :].ap, op=ALU.add), [a1[:, :], a2[:, :]], [dst])
                    else:
                        for (dv_, p0, p1) in dst:
                            pg.op("dve", lambda e, dv_=dv_, p0=p0, p1=p1: e.tensor_tensor(out=dv_.ap, in0=a1[p0:p1, :].ap, in1=a2[p0:p1, :].ap, op=ALU.add),
                                  [a1[:, :], a2[:, :]], [dv_])
                defer_q.append(tail)
            return consumer

        scope_cm = [None]

        def scope(name):
            if scope_cm[0] is not None:
                scope_cm[0].__exit__(None, None, None)
                scope_cm[0] = None
            if name is not None:
                scope_cm[0] = nc.named_scope(name)
                scope_cm[0].__enter__()

        for li, layer in enumerate(layers):
            lambda_init = 0.8 - 0.6 * math.exp(-0.3 * layer)
            scope("L%d_p0" % li)
            is_last = (li == L - 1)
            x_src = x_d if li == 0 else x1_d
            x_src_name = None if li == 0 else "D_x1"
            lpo = li * NLP

            stg = [TAB.arr(F32, 0, [DM]), SR.arr(F32, 0, [DM])]
            nwb = SR.arr(F32, 8192, [DM])
            xsb = [SR.arr(BF16, 16384, [DM]), SR.arr(BF16, 20480, [DM])]
            pg.dma("sp", "misc", nwb[:, :], dview(nwb_d[li]))
            for tt in range(NT):
                st = stg[tt % 2]
                xb = xsb[tt % 2]
                src = dview(x_src[tt * 128:(tt + 1) * 128, :], x_src_name, (tt, tt + 1))
                pg.dma("sp", "x%d" % (tt % 2), st[:, :], src)
                ss = sm[:, SM_SS + tt:SM_SS + tt + 1]
                mse = sm[:, SM_MSE + tt:SM_MSE + tt + 1]
                rstd = sm[:, SM_RSTD + tt:SM_RSTD + tt + 1]
                pg.op("act", lambda e: e.activation(out=xb[:, :].ap, in_=st[:, :].ap, func=AF.Square, accum_out=ss.ap), [st[:, :]], [xb[:, :], ss])
                pg.op("dve", lambda e: e.tensor_scalar(out=mse.ap, in0=ss.ap, scalar1=1.0 / DM, scalar2=NORM_EPS, op0=ALU.mult, op1=ALU.add), [ss], [mse])
                pg.op("pool", lambda e: e.tensor_tensor(out=rstd.ap, in0=mse.ap, in1=cs(C_NHALF).ap, op=ALU.pow), [mse, cs(C_NHALF)], [rstd])
                pg.op("dve", lambda e: e.scalar_tensor_tensor(out=xb[:, :].ap, in0=st[:, :].ap, scalar=rstd.ap, in1=nwb[:, :].ap, op0=ALU.mult, op1=ALU.mult),
                      [st[:, :], rstd, nwb[:, :]], [xb[:, :]])
                for g4 in range(4):
                    b = (tt % 2) * 4 + g4
                    pt = ps(b, BF16, [4, 128])
                    for q in range(4):
                        c = g4 * 4 + q
                        pg.op("pe", lambda e, q=q, c=c: e.transpose(out=pt[:, q, :].ap, in_=xb[:, c * 128:(c + 1) * 128].ap, identity=ident.ap),
                              [xb[:, c * 128:(c + 1) * 128], ident], [pt[:, q, :]])
                    dst = hT[:, g4 * 4:g4 * 4 + 4, tt * 128:(tt + 1) * 128]
                    if g4 % 2 == 0:
                        pg.op("act", lambda e: e.activation(out=dst.ap, in_=pt[:, :, :].ap, func=AF.Copy), [pt[:, :, :]], [dst])
                    else:
                        pg.op("dve", lambda e: e.tensor_copy(out=dst.ap, in_=pt[:, :, :].ap), [pt[:, :, :]], [dst])

            scope("L%d_tab" % li)
            lamt = SR.arr(F32, 0, [2, 64])
            lsum_a = CR.arr(F32, 7552 + 4 * SM_LAM, [2])
            lexp_a = CR.arr(F32, 7552 + 4 * SM_LAM + 8, [2])
            lsum = lsum_a[:, :]
            lexp = lexp_a[:, :]
            neglam = sm[:, SM_LAM + 4:SM_LAM + 5]
            gsc = sm[:, SM_LAM + 5:SM_LAM + 6]
            lq = lpf[:, lpo + LP_LAM:lpo + LP_LAM + 256]
            for k in range(2):
                a = lpf[:, lpo + LP_LAM + 128 * k:lpo + LP_LAM + 128 * k + 64]
                b_ = lpf[:, lpo + LP_LAM + 128 * k + 64:lpo + LP_LAM + 128 * k + 128]
                pg.op("dve", lambda e, k=k, a=a, b_=b_: e.tensor_tensor(out=lamt[:, k, :].ap, in0=a.ap, in1=b_.ap, op=ALU.mult), [a, b_], [lamt[:, k, :]])
                pg.op("dve", lambda e, k=k: e.reduce_sum(out=lsum_a[:, k:k + 1].ap, in_=lamt[:, k, :].ap, axis=mybir.AxisListType.X),
                      [lamt[:, k, :]], [lsum_a[:, k:k + 1]])
            pg.op("act", lambda e: e.activation(out=lexp.ap, in_=lsum.ap, func=AF.Exp), [lsum], [lexp])
            pg.op("dve", lambda e: e.scalar_tensor_tensor(out=neglam.ap, in0=lexp_a[:, 1:2].ap, scalar=-lambda_init, in1=lexp_a[:, 0:1].ap, op0=ALU.add, op1=ALU.subtract),
                  [lexp], [neglam])
            sub_col = lpf[:, lpo + LP_SUB:lpo + LP_SUB + 1]
            pg.op("dve", lambda e: e.tensor_scalar(out=gsc.ap, in0=sub_col.ap, scalar1=float(1.0 - lambda_init), scalar2=None, op0=ALU.mult), [sub_col], [gsc])

            build_tables(C_INVF64, C_SGN64, SR.arr(I32, 8192, [SEQ]), SR.arr(F32, 16384, [SEQ]))

            scope("L%d_conv" % li)
            cT = SR.arr(F32, 0, [SEQ])
            zp = SR.arr(F32, 8192, [SEQ + 64])
            cv = SR.arr(F32, 8192 + 8448, [SEQ])
            sgt = [SR.arr(F32, 25088, [512]), SR.arr(F32, 27136, [512])]
            for j in range(4):
                pg.op("dve", lambda e: e.memset(zp[:, 0:2].ap, 0.0), [], [zp[:, 0:2]])

                def cons_c(tb, pa):
                    d_ = cT[:, tb * 512:(tb + 1) * 512]
                    pg.op("act", lambda e: e.activation(out=d_.ap, in_=pa[:, :].ap, func=AF.Copy), [pa[:, :]], [d_])
                inproj_fm(cons_c)

                def cons_h(tb, pa, j=j):
                    z_ = zp[:, 2 + tb * 512:2 + (tb + 1) * 512]
                    c_ = cT[:, tb * 512:(tb + 1) * 512]
                    pg.op("dve", lambda e: e.tensor_tensor(out=z_.ap, in0=pa[:, :].ap, in1=c_.ap, op=ALU.mult), [pa[:, :], c_], [z_])
                    o_ = cv[:, tb * 512:(tb + 1) * 512]
                    w0 = lpf[:, lpo + LP_CONV + 3 * j + 0:lpo + LP_CONV + 3 * j + 1]
                    w1 = lpf[:, lpo + LP_CONV + 3 * j + 1:lpo + LP_CONV + 3 * j + 2]
                    w2 = lpf[:, lpo + LP_CONV + 3 * j + 2:lpo + LP_CONV + 3 * j + 3]
                    z0 = zp[:, tb * 512:(tb + 1) * 512]
                    z1 = zp[:, 1 + tb * 512:1 + (tb + 1) * 512]
                    pg.op("dve", lambda e: e.tensor_scalar(out=o_.ap, in0=z0.ap, scalar1=w0.ap, scalar2=None, op0=ALU.mult), [z0, w0], [o_])
                    pg.op("dve", lambda e: e.scalar_tensor_tensor(out=o_.ap, in0=z1.ap, scalar=w1.ap, in1=o_.ap, op0=ALU.mult, op1=ALU.add), [z1, w1, o_], [o_])
                    pg.op("dve", lambda e: e.scalar_tensor_tensor(out=o_.ap, in0=z_.ap, scalar=w2.ap, in1=o_.ap, op0=ALU.mult, op1=ALU.add), [z_, w2, o_], [o_])
                inproj_fm(cons_h)

                def cons_b(tb, pa):
                    o_ = cv[:, tb * 512:(tb + 1) * 512]
                    pg.op("dve", lambda e: e.tensor_tensor(out=o_.ap, in0=pa[:, :].ap, in1=o_.ap, op=ALU.mult), [pa[:, :], o_], [o_])
                inproj_fm(cons_b)

                def cons_g(tb, pa, j=j):
                    s_ = sgt[tb % 2]
                    o_ = cv[:, tb * 512:(tb + 1) * 512]
                    y_ = yT[:, j, tb * 512:(tb + 1) * 512]
                    pg.op("act", lambda e: e.activation(out=s_[:, :].ap, in_=pa[:, :].ap, func=AF.Silu), [pa[:, :]], [s_[:, :]])
                    pg.op("dve", lambda e: e.tensor_tensor(out=y_.ap, in0=s_[:, :].ap, in1=o_.ap, op=ALU.mult), [s_[:, :], o_], [y_])
                inproj_fm(cons_g)

            scope("L%d_diff" % li)
            dqT = SR.arr(BF16, 0, [SEQ])
            dkT = SR.arr(BF16, 4096, [SEQ])
            vh = SR.arr(BF16, 8192, [16, 128])
            gT = SR.arr(F32, 12288, [SEQ])
            ub = [SR.arr(BF16, 20480, [512]), SR.arr(BF16, 21504, [512])]
            t1 = [SR.arr(F32, 22528, [512]), SR.arr(F32, 24576, [512])]
            t2 = [SR.arr(F32, 26624, [512]), SR.arr(F32, 28672, [512])]
            ptb = [SR.arr(BF16, 30720 + 1024 * i, [512]) for i in range(4)]
            rec = SR.arr(F32, 34816, [512])
            r0 = SR.arr(F32, 36864, [512])
            r1 = SR.arr(F32, 38912, [512])
            od = SR.arr(F32, 40960, [512])
            sq = SR.arr(F32, 43008, [512])
            lnr = SR.arr(F32, 45056, [512])
            rsd = SR.arr(F32, 47104, [512])
            dq1T = SR.arr(BF16, 49152, [SEQ])
            pg.op("pool", lambda e: e.memset(dqT[64:128, :].ap, 0.0), [], [dqT[64:128, :]])
            pg.op("pool", lambda e: e.memset(dq1T[0:64, :].ap, 0.0), [], [dq1T[0:64, :]])
            dqc = [dqT, dq1T]
            rr = [r0, r1]
            pending = [None]

            def flush_pending():
                if pending[0] is not None:
                    pending[0]()
                    pending[0] = None

            for h in range(6):
                inproj_fm(rope_consumer(lambda tb: [(dqT[0:64, tb * 512:(tb + 1) * 512], 0, 64), (dq1T[64:128, tb * 512:(tb + 1) * 512], 64, 128)],
                                        sw64, ub, t1, t2, (2, 3)), banks=(0, 1))
                flush_pending()
                inproj_fm(rope_consumer(lambda tb: dkT[:, tb * 512:(tb + 1) * 512], sw64, ub, t1, t2, (2, 3)), banks=(0, 1))

                def cons_v(g4, pa):
                    d_ = vh[:, g4 * 4:g4 * 4 + 4, :]
                    pg.op("act", lambda e: e.activation(out=d_.ap, in_=pa[:, :, :].ap, func=AF.Copy), [pa[:, :, :]], [d_])
                inproj_tm(128, cons_v, banks=(0, 1))

                def cons_dg(tb, pa):
                    d_ = gT[:, tb * 512:(tb + 1) * 512]
                    pg.op("act", lambda e: e.activation(out=d_.ap, in_=pa[:, :].ap, func=AF.Silu), [pa[:, :]], [d_])
                inproj_fm(cons_dg, banks=(0, 1))
                flush_defer()

                for tb in range(NTB):
                    t0 = tb * 512
                    nsc = 4 * tb + 4
                    steps = [(c, sc) for c in range(2) for sc in range(nsc)]

                    def col0_of(sc):
                        j = sc - 4 * tb
                        return 0 if j < 0 else 128 * j

                    def emit_s(i):
                        c, sc = steps[i]
                        col0 = col0_of(sc)
                        pS = ps(i % 4, F32, [512])
                        kk = dkT[:, sc * 128:(sc + 1) * 128]
                        qq = dqc[c][:, t0 + col0:t0 + 512]
                        pg.op("pe", lambda e: e.matmul(pS[:, col0:512].ap, lhsT=kk.ap, rhs=qq.ap, start=True, stop=True), [kk, qq], [pS[:, col0:512]])
                        pt_ = ptb[i % 4]
                        pg.op("act", lambda e: e.activation(out=pt_[:, col0:512].ap, in_=pS[:, col0:512].ap, func=AF.Exp, scale=0.125),
                              [pS[:, col0:512]], [pt_[:, col0:512]])
                        if sc - 4 * tb >= 0:
                            dd = pt_[:, col0:col0 + 128]
                            pg.op("dve", lambda e: e.tensor_tensor(out=dd.ap, in0=dd.ap, in1=tri.ap, op=ALU.mult), [dd, tri], [dd])

                    def emit_pv(i):
                        c, sc = steps[i]
                        col0 = col0_of(sc)
                        po = ps(4 + c, F32, [512])
                        pd = ps(6 + c, F32, [512])
                        pt_ = ptb[i % 4]
                        vv = vh[:, sc, :]
                        pg.op("pe", lambda e: e.matmul(po[:, col0:512].ap, lhsT=vv.ap, rhs=pt_[:, col0:512].ap, start=(sc == 0), stop=(sc == nsc - 1)),
                              [vv, pt_[:, col0:512]], [po[:, col0:512]])
                        pg.op("pe", lambda e: e.matmul(pd[:, col0:512].ap, lhsT=ones_b.ap, rhs=pt_[:, col0:512].ap, start=(sc == 0), stop=(sc == nsc - 1)),
                              [ones_b, pt_[:, col0:512]], [pd[:, col0:512]])
                        if sc == nsc - 1:
                            def cepi(c=c, po=po, pd=pd):
                                pg.op("dve", lambda e: e.reciprocal(out=rec[:, :].ap, in_=pd[:, :].ap), [pd[:, :]], [rec[:, :]])
                                r_ = rr[c]
                                pg.op("dve", lambda e: e.tensor_tensor(out=r_[:, :].ap, in0=po[:, :].ap, in1=rec[:, :].ap, op=ALU.mult), [po[:, :], rec[:, :]], [r_[:, :]])
                            if tb == 0 and c == 0:
                                cq.append(cepi)
                            else:
                                cepi()

                    cq = []
                    emit_s(0)
                    emit_s(1)
                    emit_s(2)
                    for i in range(len(steps)):
                        if i + 3 < len(steps):
                            emit_s(i + 3)
                        if i == len(steps) - 1:
                            while cq:
                                cq.pop(0)()
                        emit_pv(i)
                        if i == 13:
                            flush_pending()
                    pg.op("dve", lambda e: e.scalar_tensor_tensor(out=od[:, :].ap, in0=r1[:, :].ap, scalar=neglam.ap, in1=r0[:, :].ap, op0=ALU.mult, op1=ALU.add),
                          [r1[:, :], neglam, r0[:, :]], [od[:, :]])

                    def epi(h=h, tb=tb, t0=t0):
                        pg.op("act", lambda e: e.activation(out=sq[:, :].ap, in_=od[:, :].ap, func=AF.Square), [od[:, :]], [sq[:, :]])
                        pm = ps(0, F32, [512])
                        pg.op("pe", lambda e: e.matmul(pm[:, :].ap, lhsT=ones_f.ap, rhs=sq[:, :].ap, start=True, stop=True), [ones_f, sq[:, :]], [pm[:, :]])
                        pg.op("dve", lambda e: e.tensor_scalar(out=lnr[:, :].ap, in0=pm[:, :].ap, scalar1=1.0 / 128.0, scalar2=SUBLN_EPS, op0=ALU.mult, op1=ALU.add),
                              [pm[:, :]], [lnr[:, :]])
                        pg.op("act", lambda e: e.activation(out=lnr[:, :].ap, in_=lnr[:, :].ap, func=AF.Ln), [lnr[:, :]], [lnr[:, :]])
                        pg.op("act", lambda e: e.activation(out=rsd[:, :].ap, in_=lnr[:, :].ap, func=AF.Exp, scale=-0.5), [lnr[:, :]], [rsd[:, :]])
                        pg.op("dve", lambda e: e.scalar_tensor_tensor(out=od[:, :].ap, in0=od[:, :].ap, scalar=gsc.ap, in1=rsd[:, :].ap, op0=ALU.mult, op1=ALU.mult),
                              [od[:, :], gsc, rsd[:, :]], [od[:, :]])
                        g_ = gT[:, t0:t0 + 512]
                        y_ = yT[:, 10 + h, t0:t0 + 512]
                        pg.op("dve", lambda e: e.tensor_tensor(out=y_.ap, in0=od[:, :].ap, in1=g_.ap, op=ALU.mult), [od[:, :], g_], [y_])
                    flush_pending()
                    pending[0] = epi
            flush_pending()

            scope("L%d_spin" % li)
            qT = SR.arr(BF16, 0, [6, SEQ])
            iqT = SR.arr(BF16, 24576, [4, SEQ])
            kT = SR.arr(BF16, 40960, [SEQ])
            ikT = SR.arr(BF16, 45056, [SEQ])
            vs = SR.arr(BF16, 49152, [16, 128])
            ub2 = [SR.arr(BF16, 0, [512]), SR.arr(BF16, 1024, [512])]
            t12 = [SR.arr(F32, 2048, [512]), SR.arr(F32, 4096, [512])]
            t22 = [SR.arr(F32, 6144, [512]), SR.arr(F32, 8192, [512])]
            for c in range(4):
                inproj_fm(rope_consumer(lambda tb, c=c: iqT[:, c, tb * 512:(tb + 1) * 512], sw64, ub2, t12, t22, (4, 5)))
            inproj_fm(rope_consumer(lambda tb: ikT[:, tb * 512:(tb + 1) * 512], sw64, ub2, t12, t22, (4, 5)))
            flush_defer()
            build_tables(C_INVF128, C_SGN128, SR.arr(I32, 0, [SEQ]), SR.arr(F32, 8192, [SEQ]))
            ub3 = [YT.arr(BF16, 4 * 4096, [512]), YT.arr(BF16, 4 * 4096 + 1024, [512])]
            t13 = [YT.arr(F32, 4 * 4096 + 2048, [512]), YT.arr(F32, 4 * 4096 + 4096, [512])]
            t23 = [YT.arr(F32, 4 * 4096 + 6144, [512]), YT.arr(F32, 4 * 4096 + 8192, [512])]
            for hh in range(6):
                inproj_fm(rope_consumer(lambda tb, hh=hh: qT[:, hh, tb * 512:(tb + 1) * 512], sw128, ub3, t13, t23, (4, 5)))
            inproj_fm(rope_consumer(lambda tb: kT[:, tb * 512:(tb + 1) * 512], sw128, ub3, t13, t23, (4, 5)))

            def cons_vs(g4, pa):
                d_ = vs[:, g4 * 4:g4 * 4 + 4, :]
                pg.op("act", lambda e: e.activation(out=d_.ap, in_=pa[:, :, :].ap, func=AF.Copy), [pa[:, :, :]], [d_])
            inproj_tm(128, cons_vs)
            for hh in range(6):
                def cons_sg(tb, pa, hh=hh):
                    d_ = yT[:, 4 + hh, tb * 512:(tb + 1) * 512]
                    pg.op("act", lambda e: e.activation(out=d_.ap, in_=pa[:, :].ap, func=AF.Silu), [pa[:, :]], [d_])
                inproj_fm(cons_sg)
            iwa = TAB.arr(F32, 0, [16, 8])

            def cons_iw(g4, pa):
                d_ = iwa[:, g4 * 4:g4 * 4 + 4, :]
                pg.op("act", lambda e: e.activation(out=d_.ap, in_=pa[:, :, :].ap, func=AF.Copy), [pa[:, :, :]], [d_])
            inproj_tm(8, cons_iw)

            flush_defer()
            wo_src = wout_d[li].rearrange("(mc p) n -> p mc n", p=P)
            for mc in range(11, 16):
                pg.dma("pool", "wo%d" % (mc % 4), wout[:, mc, :], dview(wo_src[:, mc, :]))

            scope("L%d_dsa" % li)
            accs = [HT.arr(F32, 0, [SEQ]), HT.arr(F32, 8192, [SEQ])]
            mb = [HT.arr(BF16, 16384, [SEQ]), HT.arr(BF16, 20480, [SEQ])]
            rl = [HT.arr(F32, 24576, [512]), HT.arr(F32, 26624, [512]), HT.arr(F32, 40960, [512]), HT.arr(F32, 43008, [512])]
            ptd = [HT.arr(BF16, 28672 + 768 * i, [3, 128]) for i in range(4)]
            ikhi = HT.arr(BF16, 32768, [SEQ])
            junkb = HT.arr(BF16, 36864, [SEQ])
            recd = TAB.arr(F32, 512, [3, 128])
            tmpd = TAB.arr(F32, 2048, [3, 128])
            bis = TAB.arr(F32, 3584, [64])
            m8 = bis[:, 0:8]
            hi_ = bis[:, 0:1]
            lo_ = bis[:, 8:9]
            rng_ = bis[:, 9:10]
            thr = bis[:, 10:11]
            ncand = bis[:, 11:12]
            ssum = bis[:, 12:13]
            ge_ = bis[:, 13:14]
            pg.op("pool", lambda e: e.memset(ikhi[0:64, :].ap, 0.0), [], [ikhi[0:64, :]])
            pg.op("pool", lambda e: e.tensor_copy(out=ikhi[64:128, :].ap, in_=ikT[64:128, :].ap), [ikT[64:128, :]], [ikhi[64:128, :]])
            pg.op("pool", lambda e: e.memset(ikT[64:128, :].ap, 0.0), [ikhi[64:128, :]], [ikT[64:128, :]])
            ikh = [ikT, ikhi]
            rl_rr = [0]

            def idx_units(qi):
                units = []
                accA = accs[qi % 2]
                nsk = qi + 1
                nkb = (nsk + 3) // 4
                for kb in range(nkb):
                    cols = min(512, 128 * nsk - 512 * kb)
                    for hh in range(8):
                        def u(kb=kb, cols=cols, hh=hh):
                            c, half = hh // 2, hh % 2
                            pL = ps(hh % 2, F32, [512])
                            qv = iqT[:, c, qi * 128:(qi + 1) * 128]
                            kv = ikh[half][:, kb * 512:kb * 512 + cols]
                            pg.op("pe", lambda e: e.matmul(pL[:, 0:cols].ap, lhsT=qv.ap, rhs=kv.ap, start=True, stop=True), [qv, kv], [pL[:, 0:cols]])
                            r_ = rl[rl_rr[0] % 4]
                            rl_rr[0] += 1
                            pg.op("act", lambda e: e.activation(out=r_[:, 0:cols].ap, in_=pL[:, 0:cols].ap, func=AF.Relu), [pL[:, 0:cols]], [r_[:, 0:cols]])
                            a_ = accA[:, kb * 512:kb * 512 + cols]
                            wv = iwa[:, qi, hh:hh + 1]
                            if hh == 0:
                                pg.op("dve", lambda e: e.tensor_scalar(out=a_.ap, in0=r_[:, 0:cols].ap, scalar1=wv.ap, scalar2=None, op0=ALU.mult), [r_[:, 0:cols], wv], [a_])
                            else:
                                pg.op("dve", lambda e: e.scalar_tensor_tensor(out=a_.ap, in0=r_[:, 0:cols].ap, scalar=wv.ap, in1=a_.ap, op0=ALU.mult, op1=ALU.add),
                                      [r_[:, 0:cols], wv, a_], [a_])
                        units.append(u)

                def ud():
                    dg = accA[:, qi * 128:(qi + 1) * 128]
                    pg.op("dve", lambda e: e.tensor_tensor(out=dg.ap, in0=dg.ap, in1=negc.ap, op=ALU.add), [dg, negc], [dg])
                units.append(ud)
                return units

            def bis_units(qi):
                units = []
                accA = accs[qi % 2]
                nk = 128 * (qi + 1)
                av = accA[:, 0:nk]
                mbq = mb[qi % 2]

                def umask():
                    pg.op("dve", lambda e: e.tensor_scalar(out=mbq[:, 0:nk].ap, in0=av.ap, scalar1=thr.ap, scalar2=MASK_NEG, op0=ALU.is_lt, op1=ALU.mult),
                          [av, thr], [mbq[:, 0:nk], junkb[:, :]])
                if qi < 2:
                    def u0():
                        pg.op("dve", lambda e: e.memset(thr.ap, -1.0e29), [], [thr])
                        umask()
                    return [u0]
                a256 = accA[:, 0:256]
                pws = cstf[:, C_PW:C_PW + NBIS]
                rs = bis[:, 16:16 + NBIS]
                jv = junkb[:, 0:nk]

                nrs = bis[:, 36:36 + NBIS]

                def usetup():
                    pg.op("dve", lambda e: e.max(out=m8.ap, in_=av.ap), [av], [m8])
                    pg.op("dve", lambda e: e.tensor_reduce(out=lo_.ap, in_=a256.ap, axis=mybir.AxisListType.X, op=ALU.min), [a256], [lo_])
                    pg.op("dve", lambda e: e.tensor_tensor(out=rng_.ap, in0=hi_.ap, in1=lo_.ap, op=ALU.subtract), [hi_, lo_], [rng_])
                    pg.op("dve", lambda e: e.tensor_scalar(out=rs.ap, in0=pws.ap, scalar1=rng_.ap, scalar2=None, op0=ALU.mult), [pws, rng_], [rs])
                    pg.op("dve", lambda e: e.tensor_scalar(out=nrs.ap, in0=rs.ap, scalar1=-1.0, scalar2=None, op0=ALU.mult), [rs], [nrs])
                    rs0 = bis[:, 16:17]
                    pg.op("dve", lambda e: e.tensor_scalar(out=ncand.ap, in0=lo_.ap, scalar1=rs0.ap, scalar2=-1.0, op0=ALU.add, op1=ALU.mult), [lo_, rs0], [ncand])
                units.append(usetup)
                for it in range(NBIS):
                    def ui(it=it):
                        nrsi = bis[:, 36 + it:37 + it]
                        rsn = bis[:, 17 + it:18 + it] if it + 1 < NBIS else bis[:, 16 + it:17 + it]
                        pg.op("act", lambda e: e.activation(out=jv.ap, in_=av.ap, func=AF.Sign, bias=ncand.ap, scale=1.0, accum_out=ssum.ap),
                              [av, ncand], [ssum])
                        pg.op("dve", lambda e: e.tensor_scalar(out=ge_.ap, in0=ssum.ap, scalar1=float(511.0 - nk), scalar2=nrsi.ap, op0=ALU.is_ge, op1=ALU.mult),
                              [ssum, nrsi], [ge_])
                        pg.op("dve", lambda e: e.scalar_tensor_tensor(out=ncand.ap, in0=ge_.ap, scalar=rsn.ap, in1=ncand.ap, op0=ALU.add, op1=ALU.add),
                              [ge_, rsn, ncand], [ncand])
                    units.append(ui)

                def ufin():
                    pg.op("dve", lambda e: e.tensor_scalar(out=thr.ap, in0=ncand.ap, scalar1=-1.0, scalar2=None, op0=ALU.mult), [ncand], [thr])
                    umask()
                units.append(ufin)
                return units

            def back_units(qi):
                units = []
                nsk = qi + 1
                mbq = mb[qi % 2]
                steps = [(g, sc) for g in range(2) for sc in range(nsk)]

                def emit_s(i):
                    g, sc = steps[i]
                    pS = ps(2 + (i % 2), F32, [3, 128])
                    kk = kT[:, sc * 128:(sc + 1) * 128]
                    qq = qT[:, 3 * g:3 * g + 3, qi * 128:(qi + 1) * 128]
                    mm = mbq[:, sc * 128:(sc + 1) * 128]
                    pg.op("pe", lambda e: e.matmul(pS[:, :, :].ap, lhsT=kk.ap, rhs=qq.ap, start=True, stop=False), [kk, qq], [pS[:, :, :]])
                    pg.op("pe", lambda e: e.matmul(pS[:, :, :].ap, lhsT=mm.ap, rhs=i3[:, :, :].ap, start=False, stop=True), [mm, i3[:, :, :]], [pS[:, :, :]])
                    pt_ = ptd[i % 4]
                    pg.op("act", lambda e: e.activation(out=pt_[:, :, :].ap, in_=pS[:, :, :].ap, func=AF.Exp, scale=float(128.0 ** -0.5)),
                          [pS[:, :, :]], [pt_[:, :, :]])

                def emit_pv(i):
                    g, sc = steps[i]
                    po = ps(4 + g, F32, [3, 128])
                    pd = ps(6 + g, F32, [3, 128])
                    pt_ = ptd[i % 4]
                    vv = vs[:, sc, :]
                    pg.op("pe", lambda e: e.matmul(po[:, :, :].ap, lhsT=vv.ap, rhs=pt_[:, :, :].ap, start=(sc == 0), stop=(sc == nsk - 1)),
                          [vv, pt_[:, :, :]], [po[:, :, :]])
                    pg.op("pe", lambda e: e.matmul(pd[:, :, :].ap, lhsT=ones_b.ap, rhs=pt_[:, :, :].ap, start=(sc == 0), stop=(sc == nsk - 1)),
                          [ones_b, pt_[:, :, :]], [pd[:, :, :]])
                    if sc == nsk - 1:
                        pg.op("act", lambda e: e.activation(out=recd[:, :, :].ap, in_=pd[:, :, :].ap, func=AF.Ln), [pd[:, :, :]], [recd[:, :, :]])
                        pg.op("act", lambda e: e.activation(out=recd[:, :, :].ap, in_=recd[:, :, :].ap, func=AF.Exp, scale=-1.0), [recd[:, :, :]], [recd[:, :, :]])
                        pg.op("dve", lambda e: e.tensor_tensor(out=tmpd[:, :, :].ap, in0=po[:, :, :].ap, in1=recd[:, :, :].ap, op=ALU.mult),
                              [po[:, :, :], recd[:, :, :]], [tmpd[:, :, :]])
                        y_ = yT[:, 4 + 3 * g:4 + 3 * g + 3, qi * 128:(qi + 1) * 128]
                        pg.op("pool", lambda e: e.tensor_tensor(out=y_.ap, in0=tmpd[:, :, :].ap, in1=y_.ap, op=ALU.mult), [tmpd[:, :, :], y_], [y_])

                for i in range(len(steps)):
                    def u(i=i):
                        if i == 0:
                            emit_s(0)
                        if i + 1 < len(steps):
                            emit_s(i + 1)
                        emit_pv(i)
                    units.append(u)
                return units

            def interleave(lists):
                lists = [l for l in lists if l]
                pos = [0] * len(lists)
                for _ in range(sum(len(l) for l in lists)):
                    k = min((k for k in range(len(lists)) if pos[k] < len(lists[k])), key=lambda k: (pos[k] + 0.5) / len(lists[k]))
                    lists[k][pos[k]]()
                    pos[k] += 1

            for r in range(-2, NT):
                streams = []
                if 0 <= r + 2 < NT:
                    streams.append(idx_units(r + 2))
                if 0 <= r + 1 < NT:
                    streams.append(bis_units(r + 1))
                if r >= 0:
                    streams.append(back_units(r))
                interleave(streams)

            if debug and li == 0:
                dbg_d = nc.dram_tensor("dbg_y", [16, P, SEQ], F32, kind="ExternalOutput").ap()
                pg.stream("dbg")
                for c in range(16):
                    pg.dma("pool", "dbg", View(dbg_d[c], []), yT[:, c, :])
            scope("L%d_out" % li)
            for mc in range(0, 11):
                pg.dma("pool", "wo%d" % (mc % 4), wout[:, mc, :], dview(wo_src[:, mc, :]))
            stgo = [SR.arr(F32, 0, [DM]), SR.arr(F32, 8192, [DM])]
            fnw = SR.arr(F32, 16384, [DM])
            junk = SR.arr(BF16, 24576, [DM])
            if is_last and final:
                pg.dma("sp", "misc", fnw[:, :], dview(fnw_d[:, :]))
            for tt in range(NT):
                st = stgo[tt % 2]
                src = dview(x_src[tt * 128:(tt + 1) * 128, :], x_src_name, (tt, tt + 1))
                pg.dma("sp", "x%d" % (tt % 2), st[:, :], src)
                for nb in range(4):
                    pa = ps((tt % 2) * 4 + nb, F32, [512])
                    for mc in range(16):
                        yv = yT[:, mc, tt * 128:(tt + 1) * 128]
                        wv = wout[:, mc, nb * 512:(nb + 1) * 512]
                        pg.op("pe", lambda e, mc=mc: e.matmul(pa[:, :].ap, lhsT=yv.ap, rhs=wv.ap, start=(mc == 0), stop=(mc == 15)), [yv, wv], [pa[:, :]])
                    s_ = st[:, nb * 512:(nb + 1) * 512]
                    pg.op("dve", lambda e: e.tensor_tensor(out=s_.ap, in0=pa[:, :].ap, in1=s_.ap, op=ALU.add), [pa[:, :], s_], [s_])
                if is_last and final:
                    ss = sm[:, SM_SS + tt:SM_SS + tt + 1]
                    mse = sm[:, SM_MSE + tt:SM_MSE + tt + 1]
                    rstd = sm[:, SM_RSTD + tt:SM_RSTD + tt + 1]
                    pg.op("act", lambda e: e.activation(out=junk[:, :].ap, in_=st[:, :].ap, func=AF.Square, accum_out=ss.ap), [st[:, :]], [ss])
                    pg.op("dve", lambda e: e.tensor_scalar(out=mse.ap, in0=ss.ap, scalar1=1.0 / DM, scalar2=NORM_EPS, op0=ALU.mult, op1=ALU.add), [ss], [mse])
                    pg.op("pool", lambda e: e.tensor_tensor(out=rstd.ap, in0=mse.ap, in1=cs(C_NHALF).ap, op=ALU.pow), [mse, cs(C_NHALF)], [rstd])
                    pg.op("dve", lambda e: e.scalar_tensor_tensor(out=st[:, :].ap, in0=st[:, :].ap, scalar=rstd.ap, in1=fnw[:, :].ap, op0=ALU.mult, op1=ALU.mult),
                          [st[:, :], rstd, fnw[:, :]], [st[:, :]])
                if is_last:
                    dst = dview(out_d[tt * 128:(tt + 1) * 128, :], "D_out", (tt, tt + 1))
                else:
                    dst = dview(x1_d[tt * 128:(tt + 1) * 128, :], "D_x1", (tt, tt + 1))
                pg.dma("sp", "st%d" % (tt % 2), dst, st[:, :])

        scope(None)
        for s in ("st0", "st1"):
            nc.sync.wait_ge(pg.sem[s], pg.cnt[s])
        print("[kernel] instr counts", {k: v for k, v in pg.cnt.items()}, "waits", pg.n_wait, flush=True)
    return nc


_PROG_CACHE = {}


def _get_prog(key):
    if key not in _PROG_CACHE:
        layers, final = key
        _PROG_CACHE[key] = build_program(list(layers), final)
    return _PROG_CACHE[key]


def _layer_inputs(layers, norm_w, w_in, conv_w, lam_q1, lam_k1, lam_q2, lam_k2, subln_w, w_out):
    L = len(layers)
    nwb = np.ascontiguousarray(np.broadcast_to(norm_w[layers][:, None, :], (L, P, DM))).astype(np.float32)
    win = np.ascontiguousarray(w_in[layers][:, :, _COLS])
    wout = np.ascontiguousarray(w_out[layers])
    lp = np.zeros((P, L * NLP), np.float32)
    for i, l in enumerate(layers):
        o = i * NLP
        lp[:, o + LP_CONV:o + LP_CONV + 12] = conv_w[l].reshape(3, 4, P).transpose(2, 1, 0).reshape(P, 12)
        lp[:, o + LP_SUB] = subln_w[l]
        lp[:, o + LP_LAM + 0:o + LP_LAM + 64] = lam_q1[l][None, :]
        lp[:, o + LP_LAM + 64:o + LP_LAM + 128] = lam_k1[l][None, :]
        lp[:, o + LP_LAM + 128:o + LP_LAM + 192] = lam_q2[l][None, :]
        lp[:, o + LP_LAM + 192:o + LP_LAM + 256] = lam_k2[l][None, :]
    return nwb, win, wout, lp


def _run(layers, final, xs, positions, norm_w, w_in, conv_w, lam_q1, lam_k1, lam_q2, lam_k2, subln_w, w_out, final_norm_w):
    nc = _get_prog((tuple(layers), final))
    nwb, win, wout, lp = _layer_inputs(layers, norm_w, w_in, conv_w, lam_q1, lam_k1, lam_q2, lam_k2, subln_w, w_out)
    cst = _consts()
    fnwb = np.ascontiguousarray(np.broadcast_to(final_norm_w[None, :], (P, DM))).astype(np.float32)
    in_maps = []
    for b in range(8):
        in_maps.append({
            "x": np.ascontiguousarray(xs[b]),
            "pos": np.ascontiguousarray(np.broadcast_to(positions[b][None, :], (P, SEQ))).astype(np.int32),
            "nwb": nwb, "w_in": win, "w_out": wout, "lp": lp, "cst": cst, "fnwb": fnwb,
        })
    res = run_bass_kernel_spmd(nc, in_maps, core_ids=list(range(8)))
    return np.stack([np.asarray(r["out"]) for r in res.results], axis=0)


FUSED = True


def kernel(x, positions, norm_w, w_in, conv_w, lam_q1, lam_k1, lam_q2, lam_k2, subln_w, w_out, final_norm_w):
    args = [np.asarray(a) for a in (positions, norm_w, w_in, conv_w, lam_q1, lam_k1, lam_q2, lam_k2, subln_w, w_out, final_norm_w)]
    x = np.asarray(x, dtype=np.float32)
    if FUSED:
        out = _run([0, 1], True, x, *args)
    else:
        x1 = _run([0], False, x, *args)
        out = _run([1], True, x1, *args)
    return out.astype(np.float32)
```
